# Optimizing a Trainium2 kernel written in Bass

```python
import math
import jax, jax.numpy as jnp
from jax import lax
import numpy as np

D_MODEL = 1024
BATCH = 8
SEQ = 4096
DEPTH = 2

N_MEM = 256
RET_HEADS = 4
RET_DK = D_MODEL // RET_HEADS
RET_DV = 2 * RET_DK
RET_CHUNK = 128
DIFF_HEADS = 4
DIFF_DK = D_MODEL // (2 * DIFF_HEADS)
DIFF_DV = 2 * DIFF_DK
Q_BLOCK = 128
CROSS_HEADS = 4
CROSS_DH = D_MODEL // CROSS_HEADS
D_FF = 4 * D_MODEL
N_BRANCH = 3
EPS = 1e-6

RET_QK_W = RET_HEADS * RET_DK
RET_V_W = RET_HEADS * RET_DV
DIFF_QK_W = DIFF_HEADS * 2 * DIFF_DK
DIFF_V_W = DIFF_HEADS * DIFF_DV
CROSS_W = CROSS_HEADS * CROSS_DH
GATE_W = N_BRANCH * D_MODEL
SPLITS = (RET_QK_W, RET_QK_W, RET_V_W, RET_V_W, DIFF_QK_W, DIFF_QK_W, DIFF_V_W, CROSS_W, GATE_W)
D_IN = RET_QK_W * 2 + RET_V_W * 2 + DIFF_QK_W * 2 + DIFF_V_W + CROSS_W + GATE_W

kernel_name = "hybrid_retention_diffattn_gated_block"


def rms_norm(x, g):
    xf = x.astype(jnp.float32)
    y = xf * lax.rsqrt(jnp.mean(xf * xf, axis=-1, keepdims=True) + EPS)
    return (y * g.astype(jnp.float32)).astype(x.dtype)


def head_norm(x, g, center):
    b, s, h, d = x.shape
    xf = x.astype(jnp.float32)
    if center:
        xf = xf - jnp.mean(xf, axis=-1, keepdims=True)
    y = xf * lax.rsqrt(jnp.mean(xf * xf, axis=-1, keepdims=True) + EPS)
    return (y.reshape(b, s, h * d) * g.astype(jnp.float32)).astype(x.dtype)


def split_cols(z, widths):
    out = []
    off = 0
    for w in widths:
        out.append(z[..., off:off + w])
        off += w
    return out


def retention_chunkwise(q, k, v):
    b, s, h, dk = q.shape
    dv = v.shape[-1]
    c = RET_CHUNK
    n = s // c
    log_g = jnp.log(1.0 - jnp.exp2(-5.0 - jnp.arange(h, dtype=jnp.float32)))
    idx = jnp.arange(c, dtype=jnp.float32)
    rel = idx[:, None] - idx[None, :]
    intra = jnp.where(rel >= 0, jnp.exp(log_g[:, None, None] * jnp.maximum(rel, 0.0)), 0.0)
    in_decay = jnp.exp(log_g[:, None] * (idx + 1.0))
    st_decay = jnp.exp(log_g[:, None] * (c - 1.0 - idx))
    ch_decay = jnp.exp(log_g * c)

    def to_chunks(t):
        return t.reshape(b, n, c, h, t.shape[-1]).transpose(1, 0, 3, 2, 4)

    qc = to_chunks(q)
    kc = to_chunks(k * (dk ** -0.5))
    vc = to_chunks(v)

    def step(state, inp):
        qi, ki, vi = inp
        scores = jnp.einsum("bhid,bhjd->bhij", qi, ki) * intra[None]
        inner = jnp.einsum("bhij,bhje->bhie", scores, vi)
        cross = jnp.einsum("bhid,bhde->bhie", qi, state) * in_decay[None, :, :, None]
        new_state = ch_decay[None, :, None, None] * state + jnp.einsum(
            "bhjd,bhje->bhde", ki * st_decay[None, :, :, None], vi)
        return new_state, inner + cross

    state0 = jnp.zeros((b, h, dk, dv), jnp.float32)
    _, out = lax.scan(step, state0, (qc, kc, vc))
    return out.transpose(1, 0, 3, 2, 4).reshape(b, s, h, dv).astype(q.dtype)


def diff_attention(q, k, v, lam):
    b, s, h, _, d = q.shape
    qt = q.transpose(0, 2, 3, 1, 4)
    kt = k.transpose(0, 2, 3, 1, 4)
    vt = v.transpose(0, 2, 1, 3)
    slopes = jnp.exp2(-8.0 * (jnp.arange(h, dtype=jnp.float32) + 1.0) / h)
    scale = d ** -0.5
    outs = []
    for i in range(s // Q_BLOCK):
        q0 = i * Q_BLOCK
        kend = q0 + Q_BLOCK
        sc = jnp.einsum("bhmqd,bhmkd->bhmqk", qt[:, :, :, q0:kend], kt[:, :, :, :kend]).astype(jnp.float32) * scale
        dist = (jnp.arange(q0, kend)[:, None] - jnp.arange(kend)[None, :]).astype(jnp.float32)
        sc = sc - (slopes[:, None, None] * dist[None])[None, :, None]
        sc = jnp.where(dist[None, None, None] >= 0, sc, -jnp.inf)
        p = jax.nn.softmax(sc, axis=-1)
        pd = p[:, :, 0] - lam * p[:, :, 1]
        outs.append(jnp.einsum("bhqk,bhke->bhqe", pd, vt[:, :, :kend]))
    o = jnp.concatenate(outs, axis=2)
    return o.transpose(0, 2, 1, 3).astype(q.dtype)


def memory_cross_attention(q, mk, mv):
    sc = jnp.einsum("bshd,bmhd->bhsm", q, mk).astype(jnp.float32) * (q.shape[-1] ** -0.5)
    p = jax.nn.softmax(sc, axis=-1)
    return jnp.einsum("bhsm,bmhd->bshd", p, mv).astype(q.dtype)


def setup_inputs(seed: int = 0) -> dict:
    key = jax.random.key(seed)
    ks = jax.random.split(key, 20)
    f32 = jnp.float32
    res_scale = (2.0 * DEPTH) ** -0.5

    def nrm(k, shape, scale):
        return jax.random.normal(k, shape, f32) * scale

    def gain(k, shape):
        return 1.0 + 0.02 * jax.random.normal(k, shape, f32)

    return {
        "x": nrm(ks[0], (BATCH, SEQ, D_MODEL), 1.0),
        "mem": nrm(ks[1], (BATCH, N_MEM, D_MODEL), 1.0),
        "g_mix": gain(ks[2], (DEPTH, D_MODEL)),
        "w_in": nrm(ks[3], (DEPTH, D_MODEL, D_IN), D_MODEL ** -0.5),
        "g_ret": gain(ks[4], (DEPTH, RET_V_W)),
        "w_ret_o": nrm(ks[5], (DEPTH, RET_V_W, D_MODEL), RET_V_W ** -0.5),
        "lambda_q1": nrm(ks[6], (DEPTH, DIFF_DK), 0.1),
        "lambda_k1": nrm(ks[7], (DEPTH, DIFF_DK), 0.1),
        "lambda_q2": nrm(ks[8], (DEPTH, DIFF_DK), 0.1),
        "lambda_k2": nrm(ks[9], (DEPTH, DIFF_DK), 0.1),
        "g_diff": gain(ks[10], (DEPTH, DIFF_V_W)),
        "w_diff_o": nrm(ks[11], (DEPTH, DIFF_V_W, D_MODEL), DIFF_V_W ** -0.5),
        "g_mem": gain(ks[12], (DEPTH, D_MODEL)),
        "w_mem_kv": nrm(ks[13], (DEPTH, D_MODEL, 2 * CROSS_W), D_MODEL ** -0.5),
        "w_cross_o": nrm(ks[14], (DEPTH, CROSS_W, D_MODEL), CROSS_W ** -0.5),
        "w_out": nrm(ks[15], (DEPTH, D_MODEL, D_MODEL), D_MODEL ** -0.5 * res_scale),
        "g_ffn": gain(ks[16], (DEPTH, D_MODEL)),
        "w_up": nrm(ks[17], (DEPTH, D_MODEL, D_FF), D_MODEL ** -0.5),
        "w_down": nrm(ks[18], (DEPTH, D_FF, D_MODEL), D_FF ** -0.5 * res_scale),
        "g_final": gain(ks[19], (D_MODEL,)),
    }


def reference(x, mem, g_mix, w_in, g_ret, w_ret_o, lambda_q1, lambda_k1, lambda_q2, lambda_k2,
              g_diff, w_diff_o, g_mem, w_mem_kv, w_cross_o, w_out, g_ffn, w_up, w_down, g_final):
    b, s, _ = x.shape
    m = mem.shape[1]
    for l in range(DEPTH):
        h = rms_norm(x, g_mix[l])
        z = h @ w_in[l]
        rq, rk, rv, rg, dq, dk, dv, cq, gates = split_cols(z, SPLITS)

        ret = retention_chunkwise(rq.reshape(b, s, RET_HEADS, RET_DK),
                                  rk.reshape(b, s, RET_HEADS, RET_DK),
                                  rv.reshape(b, s, RET_HEADS, RET_DV))
        ret = jax.nn.swish(rg) * head_norm(ret, g_ret[l], True)
        y_ret = ret @ w_ret_o[l]

        lam_init = 0.8 - 0.6 * math.exp(-0.3 * l)
        lam = (jnp.exp(jnp.sum(lambda_q1[l].astype(jnp.float32) * lambda_k1[l].astype(jnp.float32)))
               - jnp.exp(jnp.sum(lambda_q2[l].astype(jnp.float32) * lambda_k2[l].astype(jnp.float32)))
               + lam_init)
        da = diff_attention(dq.reshape(b, s, DIFF_HEADS, 2, DIFF_DK),
                            dk.reshape(b, s, DIFF_HEADS, 2, DIFF_DK),
                            dv.reshape(b, s, DIFF_HEADS, DIFF_DV), lam)
        da = head_norm(da, g_diff[l], False) * (1.0 - lam_init)
        y_diff = da @ w_diff_o[l]

        kv = rms_norm(mem, g_mem[l]) @ w_mem_kv[l]
        mk = kv[..., :CROSS_W].reshape(b, m, CROSS_HEADS, CROSS_DH)
        mv = kv[..., CROSS_W:].reshape(b, m, CROSS_HEADS, CROSS_DH)
        ca = memory_cross_attention(cq.reshape(b, s, CROSS_HEADS, CROSS_DH), mk, mv)
        y_cross = ca.reshape(b, s, CROSS_W) @ w_cross_o[l]

        g = jax.nn.sigmoid(gates.astype(jnp.float32)).astype(x.dtype).reshape(b, s, N_BRANCH, D_MODEL)
        merged = g[:, :, 0] * y_ret + g[:, :, 1] * y_diff + g[:, :, 2] * y_cross
        x = x + merged @ w_out[l]

        u = jax.nn.relu(rms_norm(x, g_ffn[l]) @ w_up[l])
        x = x + (u * u) @ w_down[l]
    return rms_norm(x, g_final)
```

```python
import math
from contextlib import ExitStack

import numpy as np
import concourse.bass as bass
import concourse.mybir as mybir
from concourse.bass_utils import run_bass_kernel_spmd

F32 = mybir.dt.float32
BF16 = mybir.dt.bfloat16
AF = mybir.ActivationFunctionType
ALU = mybir.AluOpType
AX = mybir.AxisListType

D = 1024
NMEM = 256
DIN = 13312
DFF = 4096
OFF_RQ, OFF_RK, OFF_RV, OFF_RG = 0, 1024, 2048, 4096
OFF_DQ, OFF_DK, OFF_DV, OFF_CQ, OFF_G = 6144, 7168, 8192, 9216, 10240
EPS = 1e-6
NEG = -30000.0

C_ID = 0
C_B = 128
C_CC = C_B + 4 * 1024
C_RET = C_CC + 128
C_RET_W = 641
NCONST = C_RET + 4 * C_RET_W


def make_consts():
    c = np.zeros((128, NCONST), np.float64)
    c[:, C_ID:C_ID + 128] = np.eye(128)
    kk = np.arange(128)[:, None]
    qq = np.arange(512)[None, :]
    for h in range(4):
        slope = 2.0 ** (-8.0 * (h + 1) / 4)
        b = -slope * (qq - kk)
        c[:, C_B + h * 1024:C_B + h * 1024 + 512] = b
        c[:, C_B + h * 1024 + 512:C_B + h * 1024 + 1024] = np.where(qq >= kk, b, NEG)
        for dt in range(32):
            c[:, C_CC + h * 32 + dt] = -slope * dt * 128
    a = np.arange(128)[None, :]
    for h in range(4):
        lg = math.log(1.0 - 2.0 ** (-5.0 - h))
        o = C_RET + h * C_RET_W
        c[:, o:o + 128] = np.where(a >= kk, np.exp(-lg * (kk + 1.0)) * 256 ** -0.5, 0.0)
        for rep in range(4):
            c[:, o + 128 + rep * 128:o + 256 + rep * 128] = np.exp(lg * (a + 1.0)) * np.ones((128, 1))
        c[:, o + 640] = np.exp(lg * (127.0 - kk[:, 0])) * 256 ** -0.5
    return c.astype(np.float32)


class Tl:
    __slots__ = ("name", "ap", "w", "r", "dsem", "dtot", "acc")

    def __init__(self, name, ap=None, acc=False):
        self.name = name
        self.ap = ap
        self.w = {}
        self.r = {}
        self.dsem = None
        self.dtot = 0
        self.acc = acc

    def __getitem__(self, k):
        return self.ap[k]


ENGS = ("sp", "act", "dve", "pool", "pe")


class Prog:
    def __init__(self, nc, es):
        self.nc = nc
        self.es = es
        self.streams = {e: [] for e in ENGS}
        self.tiles = []
        self.esem = {e: es.enter_context(nc.semaphore("es_" + e)) for e in ENGS}
        self.dsems = []
        self.semh = []
        self.semtot = []
        self.semfree = []
        self.semeng = []
        self.lastc = {e: -1 for e in ENGS}

    def tile(self, name, ap=None, acc=False):
        t = Tl(name, ap, acc)
        self.tiles.append(t)
        return t

    def sb(self, name, shape, dt):
        h = self.es.enter_context(self.nc.sbuf_tensor(name, list(shape), dt))
        return self.tile(name, h[:] if len(shape) == 2 else h[tuple(slice(None) for _ in shape)])

    @staticmethod
    def _merge(d, s):
        for k, v in s.items():
            if d.get(k, -1) < v:
                d[k] = v

    def _deps(self, eng, R, W, is_dma):
        raw, oth = {}, {}
        for t in R:
            self._merge(raw, t.w)
        for t in W:
            if not t.acc:
                self._merge(oth, t.w)
            self._merge(oth, t.r)
        own = ("c", eng)
        if not is_dma and eng == "pe":
            oth.pop(own, None)
            raw.pop(own, None)
        self._merge(raw, oth)
        return raw

    def add(self, eng, fn, R=(), W=()):
        deps = self._deps(eng, R, W, False)
        idx = len(self.streams[eng])
        self.streams[eng].append([fn, deps, None])
        self.lastc[eng] = idx
        key = ("c", eng)
        for t in R:
            if t.r.get(key, -1) < idx:
                t.r[key] = idx
        for t in W:
            t.w = {key: idx}
            t.r = {}
        return idx

    def dma(self, eng, out, in_, R=(), W=(), st=None, slow=False):
        if st is None:
            st = [t for t in list(W) + list(R) if t.ap is not None][0]
        if st.dsem is not None and self.semeng[st.dsem] != eng:
            raise RuntimeError("tile %s used by DMAs of two queues" % st.name)
        if st.dsem is None:
            fl = [i for i in self.semfree if self.semeng[i] == eng]
            if fl:
                st.dsem = fl[-1]
                self.semfree.remove(st.dsem)
            else:
                st.dsem = len(self.semh)
                self.semh.append(self.es.enter_context(self.nc.semaphore("ds%d" % st.dsem)))
                self.semtot.append(0)
                self.semeng.append(eng)
            self.dsems.append(st)
        deps = self._deps(eng, R, W, True)
        si = st.dsem
        self.semtot[si] += 16
        tot = self.semtot[si]
        key = ("d", si)
        if slow:
            fn = lambda e, o=out, i=in_: e.dma_start(out=o, in_=i, allow_slow_non_contiguous=True)
        else:
            fn = lambda e, o=out, i=in_: e.dma_start(out=o, in_=i)
        self.streams[eng].append([fn, deps, (self.semh[si], 16)])
        for t in R:
            t.r[key] = tot
        for t in W:
            if t.acc:
                t.w[key] = tot
            else:
                t.w = {key: tot}
                t.r = {}

    def barrier(self):
        last = dict(self.lastc)
        dd = {("d", i): v for i, v in enumerate(self.semtot) if v > 0}
        for e in ENGS:
            deps = dict(dd)
            for e2 in ENGS:
                if e2 != e and last[e2] >= 0:
                    deps[("c", e2)] = last[e2]
            self.streams[e].append([None, deps, None])
        for t in self.tiles:
            t.w = {}
            t.r = {}
        for t in self.dsems:
            t.dsem = None
        self.dsems = []
        self.semfree = list(range(len(self.semh)))

    def emit(self):
        nc = self.nc
        need = {e: set() for e in ENGS}
        for e in ENGS:
            for fn, deps, _ in self.streams[e]:
                for k, v in deps.items():
                    if k[0] == "c":
                        need[k[1]].add(v)
        cnt = {}
        for e in ENGS:
            c = 0
            arr = []
            for i in range(len(self.streams[e])):
                if i in need[e]:
                    c += 1
                arr.append(c)
            cnt[e] = arr
        engobj = {"sp": nc.sync, "act": nc.scalar, "dve": nc.vector, "pool": nc.gpsimd, "pe": nc.tensor}
        semof = self.semh

        def run(e):
            def body(eng):
                waited = {}
                for i, (fn, deps, dinc) in enumerate(self.streams[e]):
                    for k, v in deps.items():
                        if k[0] == "c":
                            sem, val = self.esem[k[1]], cnt[k[1]][v]
                        else:
                            sem, val = semof[k[1]], v
                        sk = id(sem)
                        if waited.get(sk, 0) < val:
                            eng.wait_ge(sem, val)
                            waited[sk] = val
                    if fn is None:
                        if i in need[e]:
                            eng.nop().then_inc(self.esem[e], 1)
                        continue
                    ins = fn(eng)
                    if dinc is not None:
                        ins.then_inc(dinc[0], dinc[1])
                    elif i in need[e]:
                        ins.then_inc(self.esem[e], 1)
            return body

        with nc.Block() as block:
            block.sync(run("sp"))
            block.scalar(run("act"))
            block.vector(run("dve"))
            block.gpsimd(run("pool"))
            block.tensor(run("pe"))


def f_mm(out, lhsT, rhs, start, stop):
    return lambda e: e.matmul(out, lhsT, rhs, start=start, stop=stop)


def f_tr(out, in_, ident):
    return lambda e: e.transpose(out=out, in_=in_, identity=ident)


def f_act(out, in_, func, **kw):
    return lambda e: e.activation(out=out, in_=in_, func=func, **kw)


def f_copy(out, in_):
    return lambda e: e.tensor_copy(out, in_)


def f_tt(out, a, b, op):
    return lambda e: e.tensor_tensor(out=out, in0=a, in1=b, op=op)


def f_ts(out, a, s1, s2, op0, op1=None):
    if op1 is None:
        return lambda e: e.tensor_scalar(out=out, in0=a, scalar1=s1, scalar2=None, op0=op0)
    return lambda e: e.tensor_scalar(out=out, in0=a, scalar1=s1, scalar2=s2, op0=op0, op1=op1)


def f_stt(out, a, s, b, op0, op1):
    return lambda e: e.scalar_tensor_tensor(out=out, in0=a, scalar=s, in1=b, op0=op0, op1=op1)


def f_memset(ap, v):
    return lambda e: e.memset(ap, v)


class _Stop(Exception):
    pass


def build(S, NL, dbg=False, stop=None):
    NT = S // 128
    NB = S // 512
    nc = bass.Bass("TRN2", target_bir_lowering=False)
    es = ExitStack()
    P = Prog(nc, es)

    def din(name, shape, dt=F32):
        return nc.dram_tensor(name, list(shape), dt, kind="ExternalInput").ap()

    x_in = din("x", [S, D])
    mem_in = din("mem", [NMEM, D])
    g_mix = din("g_mix", [2, D])
    w_in = din("w_in", [2, D, DIN])
    g_ret = din("g_ret", [2, 2048])
    w_ret_o = din("w_ret_o", [2, 2048, D])
    lq1 = din("lambda_q1", [2, 128])
    lk1 = din("lambda_k1", [2, 128])
    lq2 = din("lambda_q2", [2, 128])
    lk2 = din("lambda_k2", [2, 128])
    g_diff = din("g_diff", [2, D])
    w_diff_o = din("w_diff_o", [2, D, D])
    g_mem = din("g_mem", [2, D])
    w_mem_kv = din("w_mem_kv", [2, D, 2048])
    w_cross_o = din("w_cross_o", [2, D, D])
    w_out = din("w_out", [2, D, D])
    g_ffn = din("g_ffn", [2, D])
    w_up = din("w_up", [2, D, DFF])
    w_down = din("w_down", [2, DFF, D])
    g_final = din("g_final", [1, D])
    consts = din("consts", [128, NCONST])
    y_out = nc.dram_tensor("y", [S, D], F32, kind="ExternalOutput").ap()

    kind_s = "ExternalOutput" if dbg else "Internal"
    xres = nc.dram_tensor("xres", [S, D], F32, kind=kind_s).ap()
    retT = nc.dram_tensor("retT", [2048, S], BF16, kind=kind_s).ap()
    daT = nc.dram_tensor("daT", [1024, S], BF16, kind=kind_s).ap()
    caT = nc.dram_tensor("caT", [1024, S], BF16, kind=kind_s).ap()
    gT = nc.dram_tensor("gT", [3072, S], BF16, kind=kind_s).ap()
    d_xres = P.tile("d_xres", acc=True)
    d_retT = P.tile("d_retT", acc=True)
    d_daT = P.tile("d_daT", acc=True)
    d_caT = P.tile("d_caT", acc=True)
    d_gT = P.tile("d_gT", acc=True)
    d_y = P.tile("d_y", acc=True)

    ps = []
    for i in range(8):
        h = es.enter_context(nc.psum_tensor("ps%d" % i, [128, 512], F32))
        ps.append(P.tile("ps%d" % i, h[:]))
    ALLB = list(range(8))
    rr = {}

    def bank(pool=None):
        pool = tuple(pool or ALLB)
        i = rr.get(pool, -1) + 1
        rr[pool] = i
        return ps[pool[i % len(pool)]]

    def psb(t):
        return t.ap.bitcast(BF16)

    identf = P.sb("identf", [128, 128], F32)
    ident = P.sb("ident", [128, 128], BF16)
    epst = P.sb("epst", [128, 1], F32)
    lamt = P.sb("lamt", [128, 8], F32)
    junk = P.sb("junk", [128, 1024], BF16)
    gbc = P.sb("gbc", [128, 1024], F32)
    xts = [P.sb("xt%d" % i, [128, 1024], F32) for i in range(3)]
    sst = [P.sb("ss%d" % i, [128, 4], F32) for i in range(3)]
    hbs = [P.sb("hb%d" % i, [128, 1024], BF16) for i in range(2)]
    NAR = 93696
    ARh = es.enter_context(nc.sbuf_tensor("arena", [128, NAR], BF16))
    AR = ARh[:]
    o2 = [0]

    def carve(name, n, dt=BF16):
        if dt == F32:
            a = AR[:, o2[0]:o2[0] + 2 * n].bitcast(F32)
            o2[0] += 2 * n
        else:
            a = AR[:, o2[0]:o2[0] + n]
            o2[0] += n + (n & 1)
        assert o2[0] <= NAR - 4096, (name, o2[0])
        return P.tile(name, a)

    evac_rr = [0]

    def evac_eng():
        evac_rr[0] ^= 1
        return "act" if evac_rr[0] else "dve"

    def copy_on(eng, out, in_, R, W):
        if eng == "act":
            P.add("act", f_act(out, in_, AF.Copy), R=R, W=W)
        else:
            P.add(eng, f_copy(out, in_), R=R, W=W)

    P.dma("sp", identf[:], consts[:, C_ID:C_ID + 128], W=[identf])
    P.add("dve", f_copy(ident[:], identf[:]), R=[identf], W=[ident])
    P.add("dve", f_memset(epst[:], EPS), W=[epst])

    def rstd_from(ss, src_ap, n, R):
        P.add("act", f_act(ss[:, 1:2], src_ap, AF.Ln, scale=1.0 / n, bias=epst[:, 0:1]), R=R + [epst], W=[ss])
        P.add("act", f_act(ss[:, 2:3], ss[:, 1:2], AF.Exp, scale=-0.5), R=[ss], W=[ss])

    def norm_tile(xt, ss, gb, hb, n=1024):
        P.add("act", f_act(junk[:, 0:n], xt[:, 0:n], AF.Square, accum_out=ss[:, 0:1]), R=[xt], W=[junk, ss])
        rstd_from(ss, ss[:, 0:1], n, [ss])
        P.add("dve", f_stt(hb[:, 0:n], xt[:, 0:n], ss[:, 2:3], gb[:, 0:n], ALU.mult, ALU.mult), R=[xt, ss, gb], W=[hb])

    def transpose_to(hb, nchunk, dst_ap, dst_tile, eng=None, pool=None):
        b = bank(pool)
        pb = psb(b)
        for c in range(nchunk):
            P.add("pe", f_tr(pb[:, c * 128:(c + 1) * 128], hb[:, c * 128:(c + 1) * 128], ident[:]), R=[hb, ident], W=[b])
        copy_on(eng or evac_eng(), dst_ap, pb[:, 0:nchunk * 128].rearrange("p (c t) -> p c t", c=nchunk), [b], [dst_tile])

    wstage = [P.tile("wstage%d" % i, AR[:, NAR - 4096 + i * 2048:NAR - 4096 + (i + 1) * 2048].bitcast(F32)) for i in range(2)]
    wsi = [0]

    def load_w(tile, dst_ap, w_l, c0, n):
        src = w_l.rearrange("(kc p) n -> p kc n", p=128)
        if n >= 1024:
            pieces = [(kc, 1, cs, 1024) for kc in range(8) for cs in range(0, n, 1024)]
        else:
            kcn = 1024 // n
            pieces = [(kc, kcn, 0, n) for kc in range(0, 8, kcn)]
        for (kc0, kcn, cs, ncol) in pieces:
            st = wstage[wsi[0] % 2]
            wsi[0] += 1
            st_ap = st[:, 0:kcn * ncol].rearrange("p (k n) -> p k n", k=kcn)
            P.dma("sp", st_ap, src[:, kc0:kc0 + kcn, c0 + cs:c0 + cs + ncol], W=[st])
            copy_on(evac_eng(), dst_ap[:, kc0:kc0 + kcn, cs:cs + ncol], st_ap, [st], [tile])

    def _phase_end(k):
        P.barrier()
        if stop == k:
            raise _Stop()

    for l in range(NL):
      try:
        xsrc = x_in if l == 0 else xres
        lam_init = 0.8 - 0.6 * math.exp(-0.3 * l)
        hT_ap = AR[:, 0:8 * S].rearrange("p (c t) -> p c t", c=8)
        hTt = [P.tile("hT%d" % t) for t in range(NT)]
        P.dma("sp", gbc[:], g_mix[l:l + 1, :].broadcast_to([128, D]), W=[gbc])
        for t in range(NT):
            xt, ss, hb = xts[t % 3], sst[t % 3], hbs[t % 2]
            P.dma("sp", xt[:], xsrc[t * 128:(t + 1) * 128, :], R=[d_xres], W=[xt])
            norm_tile(xt, ss, gbc, hb)
            transpose_to(hb, 8, hT_ap[:, :, t * 128:(t + 1) * 128], hTt[t])

        def hT_blk(kc, tb):
            return hT_ap[:, kc, tb * 512:(tb + 1) * 512], hTt[tb * 4:(tb + 1) * 4]

        def hT_tok(kc, t):
            return hT_ap[:, kc, t * 128:(t + 1) * 128], [hTt[t]]

        o2[0] = 8 * S
        lt = carve("lt", 512, F32)
        lt2 = carve("lt2", 256, F32)
        for i, srcl in enumerate((lq1, lk1, lq2, lk2)):
            P.dma("sp", lt[:, i * 128:(i + 1) * 128], srcl[l:l + 1, :].broadcast_to([128, 128]), W=[lt])
        P.add("dve", f_tt(lt2[:, 0:128], lt[:, 0:128], lt[:, 128:256], ALU.mult), R=[lt], W=[lt2])
        P.add("dve", f_tt(lt2[:, 128:256], lt[:, 256:384], lt[:, 384:512], ALU.mult), R=[lt], W=[lt2])
        P.add("dve", lambda e: e.reduce_sum(out=lamt[:, 0:2], in_=lt2[:, 0:256].rearrange("p (a b) -> p a b", a=2), axis=AX.X), R=[lt2], W=[lamt])
        P.add("act", f_act(lamt[:, 2:4], lamt[:, 0:2], AF.Exp), R=[lamt], W=[lamt])
        P.add("dve", f_tt(lamt[:, 4:5], lamt[:, 2:3], lamt[:, 3:4], ALU.subtract), R=[lamt], W=[lamt])
        P.add("dve", f_ts(lamt[:, 5:6], lamt[:, 4:5], -1.0, -lam_init, ALU.mult, ALU.add), R=[lamt], W=[lamt])
        _phase_end(0)

        o2[0] = 8 * S
        mkT = carve("mkT", 8 * 256)
        mkT_ap = mkT[:, :].rearrange("p (c t) -> p c t", c=8)
        mv = carve("mv", 2 * 4 * 258)
        mv_ap = mv[:, :].rearrange("p (m h e) -> p m h e", m=2, h=4)
        o_mix = o2[0]
        mhT = carve("mhT", 8 * 256)
        mhT_ap = mhT[:, :].rearrange("p (c t) -> p c t", c=8)
        wkv = carve("wkv", 8 * 1024)
        wkv_ap = wkv[:, :].rearrange("p (c n) -> p c n", c=8)
        P.dma("sp", gbc[:], g_mem[l:l + 1, :].broadcast_to([128, D]), W=[gbc])
        for t in range(2):
            xt, ss, hb = xts[t % 3], sst[t % 3], hbs[t % 2]
            P.dma("sp", xt[:], mem_in[t * 128:(t + 1) * 128, :], W=[xt])
            norm_tile(xt, ss, gbc, hb)
            transpose_to(hb, 8, mhT_ap[:, :, t * 128:(t + 1) * 128], mhT, eng="act")
        if stop == 0.5:
            _phase_end(0.5)
        P.add("dve", f_memset(mv[:, :], 1.0), W=[mv])
        load_w(wkv, wkv_ap, w_mem_kv[l], 0, 1024)
        if stop == 0.6:
            _phase_end(0.6)
        for cc in range(8):
            b = bank()
            for kc in range(8):
                P.add("pe", f_mm(b[:, 0:256], wkv_ap[:, kc, cc * 128:(cc + 1) * 128], mhT_ap[:, kc, :], kc == 0, kc == 7), R=[wkv, mhT], W=[b])
            copy_on("act", mkT_ap[:, cc, :], b[:, 0:256], [b], [mkT])
        if stop == 0.7:
            _phase_end(0.7)
        load_w(wkv, wkv_ap, w_mem_kv[l], 1024, 1024)
        for mt in range(2):
            for half in range(2):
                b = bank()
                for kc in range(8):
                    P.add("pe", f_mm(b[:, 0:512], mhT_ap[:, kc, mt * 128:(mt + 1) * 128], wkv_ap[:, kc, half * 512:(half + 1) * 512], kc == 0, kc == 7), R=[wkv, mhT], W=[b])
                copy_on("dve", mv_ap[:, mt, 2 * half:2 * half + 2, 0:256], b[:, 0:512].rearrange("p (h e) -> p h e", h=2), [b], [mv])
        _phase_end(1)

        o2[0] = o_mix
        wq_c = [carve("wq_c%d" % i, 8 * 256) for i in range(2)]
        cqT = carve("cqT", 2 * S)
        cqT_ap = cqT[:, :].rearrange("p (c t) -> p c t", c=2)
        pts = [carve("pt%d" % i, 512) for i in range(4)]
        oq = [carve("oq%d" % i, 256) for i in range(2)]
        rsm = [carve("rsm%d" % i, 4, F32) for i in range(4)]
        stg = [carve("stg%d" % i, 2 * 512) for i in range(2)]
        sc_c = 256 ** -0.5
        for h in range(4):
            wq = wq_c[h % 2]
            wq_ap = wq[:, :].rearrange("p (c n) -> p c n", c=8)
            load_w(wq, wq_ap, w_in[l], OFF_CQ + h * 256, 256)
            for tb in range(NB):
                for c in range(2):
                    b = bank()
                    for kc in range(8):
                        a, tl = hT_blk(kc, tb)
                        P.add("pe", f_mm(b[:, 0:512], wq_ap[:, kc, c * 128:(c + 1) * 128], a, kc == 0, kc == 7), R=[wq] + tl, W=[b])
                    copy_on(evac_eng(), cqT_ap[:, c, tb * 512:(tb + 1) * 512], b[:, 0:512], [b], [cqT])
            ptq = {}

            def ca_scores(qb):
                pt2 = []
                for mt in range(2):
                    b = bank()
                    for c in range(2):
                        P.add("pe", f_mm(b[:, 0:512], mkT_ap[:, h * 2 + c, mt * 128:(mt + 1) * 128], cqT_ap[:, c, qb * 512:(qb + 1) * 512], c == 0, c == 1), R=[mkT, cqT], W=[b])
                    pt = pts[(qb * 2 + mt) % 4]
                    P.add("act", f_act(pt[:, 0:512], b[:, 0:512], AF.Exp, scale=sc_c), R=[b], W=[pt])
                    pt2.append(pt)
                ptq[qb] = pt2

            def ca_pv(qb):
                st_ = stg[qb % 2]
                st_ap = st_[:, :].rearrange("p (c t) -> p c t", c=2)
                pt2 = ptq.pop(qb)
                for qt in range(4):
                    b = bank()
                    for mt in range(2):
                        P.add("pe", f_mm(b[:, 0:258], pt2[mt][:, qt * 128:(qt + 1) * 128], mv_ap[:, mt, h, :], mt == 0, mt == 1), R=[pt2[mt], mv], W=[b])
                    rs = rsm[qt]
                    P.add("dve", lambda e, o=rs[:, 0:1], i=b[:, 256:257]: e.reciprocal(out=o, in_=i), R=[b], W=[rs])
                    o_ = oq[qt % 2]
                    P.add("dve", f_ts(o_[:, 0:256], b[:, 0:256], rs[:, 0:1], None, ALU.mult), R=[b, rs], W=[o_])
                    b2 = bank()
                    pb = psb(b2)
                    for c in range(2):
                        P.add("pe", f_tr(pb[:, c * 128:(c + 1) * 128], o_[:, c * 128:(c + 1) * 128], ident[:]), R=[o_, ident], W=[b2])
                    copy_on("act", st_ap[:, :, qt * 128:(qt + 1) * 128], pb[:, 0:256].rearrange("p (c t) -> p c t", c=2), [b2], [st_])
                dst = caT[h * 256:(h + 1) * 256, qb * 512:(qb + 1) * 512].rearrange("(c p) t -> p c t", p=128)
                P.dma("sp", dst, st_ap, R=[st_], W=[d_caT])

            ca_scores(0)
            for qb in range(NB):
                if qb + 1 < NB:
                    ca_scores(qb + 1)
                ca_pv(qb)
        _phase_end(2)

        o2[0] = o_mix
        wg_c = [carve("wg%d" % i, 8 * 512) for i in range(2)]
        gst = [carve("gst%d" % i, 2048) for i in range(2)]
        gex = [carve("gex%d" % i, 512, F32) for i in range(2)]
        gxi = [0]
        gi = 0
        for grp in range(6):
            wg = wg_c[grp % 2]
            wg_ap = wg[:, :].rearrange("p (c n) -> p c n", c=8)
            load_w(wg, wg_ap, w_in[l], OFF_G + grp * 512, 512)
            for cc in range(4):
                for tb4 in range((NB + 3) // 4):
                    g_ = gst[gi % 2]
                    gi += 1
                    nb_here = min(4, NB - tb4 * 4)
                    for tbi in range(nb_here):
                        tb = tb4 * 4 + tbi
                        b = bank()
                        for kc in range(8):
                            a, tl = hT_blk(kc, tb)
                            P.add("pe", f_mm(b[:, 0:512], wg_ap[:, kc, cc * 128:(cc + 1) * 128], a, kc == 0, kc == 7), R=[wg] + tl, W=[b])
                        ge = gex[gxi[0] % 2]
                        gxi[0] += 1
                        P.add("act", f_act(ge[:, 0:512], b[:, 0:512], AF.Exp, scale=-1.0), R=[b], W=[ge])
                        P.add("act", f_act(ge[:, 0:512], ge[:, 0:512], AF.Ln, bias=1.0), R=[ge], W=[ge])
                        P.add("act", f_act(g_[:, tbi * 512:(tbi + 1) * 512], ge[:, 0:512], AF.Exp, scale=-1.0), R=[ge], W=[g_])
                    row = grp * 512 + cc * 128
                    P.dma("sp", gT[row:row + 128, tb4 * 2048:tb4 * 2048 + nb_here * 512], g_[:, 0:nb_here * 512], R=[g_], W=[d_gT])
        _phase_end(3)
        if stop == 3.5:
            _phase_end(3.5)

        o2[0] = o_mix
        wr_c = carve("wr", 8 * 1536)
        wr_ap = wr_c[:, :].rearrange("p (c n) -> p c n", c=8)
        rc = carve("rc", C_RET_W + 1, F32)
        qk_blk = [[carve("qk%d_%d" % (i, c), 512) for c in range(4)] for i in range(2)]
        kd_sb = [carve("kd_sb%d" % i, 256) for i in range(2)]
        v_sb = [carve("v_sb%d" % i, 512) for i in range(2)]
        sg_sb = [carve("sg_sb%d" % i, 512) for i in range(2)]
        xs_sb = [carve("xs_sb%d" % i, 512, F32) for i in range(2)]
        ex_sb = [carve("ex_sb%d" % i, 512, F32) for i in range(2)]
        pT_sb = [carve("pT_sb%d" % i, 128) for i in range(2)]
        Sf = [carve("Sf%d" % i, 512, F32) for i in range(2)]
        Sb = [carve("Sb%d" % i, 512) for i in range(2)]
        yn = [carve("yn%d" % i, 512, F32) for i in range(2)]
        yg = [carve("yg%d" % i, 512) for i in range(2)]
        bst = [carve("bst%d" % i, 8, F32) for i in range(2)]
        rstg = [carve("rstg%d" % i, 4 * 512) for i in range(2)]
        for h in range(4):
            lg = math.log(1.0 - 2.0 ** (-5.0 - h))
            chd = math.exp(lg * 128.0)
            load_w(wr_c, wr_ap[:, :, 0:256], w_in[l], OFF_RQ + h * 256, 256)
            load_w(wr_c, wr_ap[:, :, 256:512], w_in[l], OFF_RK + h * 256, 256)
            load_w(wr_c, wr_ap[:, :, 512:1024], w_in[l], OFF_RV + h * 512, 512)
            load_w(wr_c, wr_ap[:, :, 1024:1536], w_in[l], OFF_RG + h * 512, 512)
            P.dma("sp", rc[:, 0:C_RET_W], consts[:, C_RET + h * C_RET_W:C_RET + (h + 1) * C_RET_W], W=[rc])
            M_ap = rc[:, 0:128]
            R4_ap = rc[:, 128:640]
            sd_ap = rc[:, 640:641]

            def qk_proj(tb):
                for c in range(4):
                    b = bank()
                    for kc in range(8):
                        a, tl = hT_blk(kc, tb)
                        P.add("pe", f_mm(b[:, 0:512], wr_ap[:, kc, c * 128:(c + 1) * 128], a, kc == 0, kc == 7), R=[wr_c] + tl, W=[b])
                    dst = qk_blk[tb % 2][c]
                    if c < 2:
                        P.add("dve", f_tt(dst[:, 0:512], b[:, 0:512], R4_ap, ALU.mult), R=[b, rc], W=[dst])
                    else:
                        copy_on("act", dst[:, 0:512], b[:, 0:512], [b], [dst])

            def ret_proj(t):
                i = t % 2
                if stop == 3.56:
                    _phase_end(3.56)
                b = bank()
                for kc in range(8):
                    a, tl = hT_tok(kc, t)
                    P.add("pe", f_mm(b[:, 0:256], a, wr_ap[:, kc, 256:512], kc == 0, kc == 7), R=[wr_c] + tl, W=[b])
                P.add("dve", f_ts(kd_sb[i][:, 0:256], b[:, 0:256], sd_ap, None, ALU.mult), R=[b, rc], W=[kd_sb[i]])
                if stop == 3.57:
                    _phase_end(3.57)
                b = bank()
                for kc in range(8):
                    a, tl = hT_tok(kc, t)
                    P.add("pe", f_mm(b[:, 0:512], a, wr_ap[:, kc, 512:1024], kc == 0, kc == 7), R=[wr_c] + tl, W=[b])
                copy_on("dve", v_sb[i][:, 0:512], b[:, 0:512], [b], [v_sb[i]])
                if stop == 3.58:
                    _phase_end(3.58)
                b = bank()
                for kc in range(8):
                    a, tl = hT_tok(kc, t)
                    P.add("pe", f_mm(b[:, 0:512], a, wr_ap[:, kc, 1024:1536], kc == 0, kc == 7), R=[wr_c] + tl, W=[b])
                P.add("act", f_act(ex_sb[i][:, 0:512], b[:, 0:512], AF.Exp, scale=-1.0), R=[b], W=[ex_sb[i]])
                P.add("act", f_act(ex_sb[i][:, 0:512], ex_sb[i][:, 0:512], AF.Ln, bias=1.0), R=[ex_sb[i]], W=[ex_sb[i]])
                P.add("act", f_act(ex_sb[i][:, 0:512], ex_sb[i][:, 0:512], AF.Exp, scale=-1.0), R=[ex_sb[i]], W=[ex_sb[i]])
                P.add("dve", f_tt(sg_sb[i][:, 0:512], b[:, 0:512], ex_sb[i][:, 0:512], ALU.mult), R=[b, ex_sb[i]], W=[sg_sb[i]])

            if stop == 3.55:
                _phase_end(3.55)
            qk_proj(0)
            ret_proj(0)
            if stop == 3.6:
                _phase_end(3.6)
            for t in range(NT):
                i = t % 2
                qb_ = qk_blk[(t // 4) % 2]
                tsl = slice((t % 4) * 128, (t % 4 + 1) * 128)
                b = bank()
                for c in range(2):
                    P.add("pe", f_mm(b[:, 0:128], qb_[2 + c][:, tsl], qb_[c][:, tsl], c == 0, c == 1), R=[qb_[2 + c], qb_[c]], W=[b])
                P.add("dve", f_tt(pT_sb[i][:, 0:128], b[:, 0:128], M_ap, ALU.mult), R=[b, rc], W=[pT_sb[i]])
                if t + 1 < NT:
                    ret_proj(t + 1)
                if t % 4 == 1 and t // 4 + 1 < NB:
                    qk_proj(t // 4 + 1)
                bo = bank()
                P.add("pe", f_mm(bo[:, 0:512], pT_sb[i][:, 0:128], v_sb[i][:, 0:512], True, t == 0), R=[pT_sb[i], v_sb[i]], W=[bo])
                if t > 0:
                    for c in range(2):
                        P.add("pe", f_mm(bo[:, 0:512], qb_[c][:, tsl], Sb[c][:, 0:512], False, c == 1), R=[qb_[c], Sb[c]], W=[bo])
                if t + 1 < NT:
                    for c in range(2):
                        bs = bank()
                        P.add("pe", f_mm(bs[:, 0:512], kd_sb[i][:, c * 128:(c + 1) * 128], v_sb[i][:, 0:512], True, True), R=[kd_sb[i], v_sb[i]], W=[bs])
                        if t == 0:
                            copy_on("dve", Sf[c][:, 0:512], bs[:, 0:512], [bs], [Sf[c]])
                        else:
                            P.add("dve", f_stt(Sf[c][:, 0:512], Sf[c][:, 0:512], chd, bs[:, 0:512], ALU.mult, ALU.add), R=[bs, Sf[c]], W=[Sf[c]])
                        copy_on("act", Sb[c][:, 0:512], Sf[c][:, 0:512], [Sf[c]], [Sb[c]])
                if stop == 3.7:
                    _phase_end(3.7)
                st6 = bst[i]
                P.add("dve", lambda e, o=st6[:, 0:6], a=bo[:, 0:512]: e.bn_stats(out=o, in_=a), R=[bo], W=[st6])
                P.add("dve", lambda e, o=st6[:, 6:8], a=st6[:, 0:6]: e.bn_aggr(out=o, in_=a), R=[st6], W=[st6])
                ss = sst[t % 3]
                rstd_from(ss, st6[:, 7:8], 1, [st6])
                P.add("dve", f_ts(yn[i][:, 0:512], bo[:, 0:512], st6[:, 6:7], ss[:, 2:3], ALU.subtract, ALU.mult), R=[bo, st6, ss], W=[yn[i]])
                P.add("dve", f_tt(yg[i][:, 0:512], yn[i][:, 0:512], sg_sb[i][:, 0:512], ALU.mult), R=[yn[i], sg_sb[i]], W=[yg[i]])
                if stop == 3.8:
                    _phase_end(3.8)
                rs_ = rstg[(t // 4) % 2]
                rs_ap = rs_[:, :].rearrange("p (c t) -> p c t", c=4)
                transpose_to(yg[i], 4, rs_ap[:, :, (t % 4) * 128:(t % 4 + 1) * 128], rs_, eng="act")
                if t % 4 == 3:
                    tb = t // 4
                    dst = retT[h * 512:(h + 1) * 512, tb * 512:(tb + 1) * 512].rearrange("(c p) t -> p c t", p=128)
                    P.dma("sp", dst, rs_ap, R=[rs_], W=[d_retT])
        _phase_end(4)

        o2[0] = o_mix
        wd_c = carve("wd", 8 * 768)
        wd_ap = wd_c[:, :].rearrange("p (c n) -> p c n", c=8)
        dqT = carve("dqT", 2 * S)
        dkT = carve("dkT", 2 * S)
        dqT_ap = dqT[:, :].rearrange("p (m t) -> p m t", m=2)
        dkT_ap = dkT[:, :].rearrange("p (m t) -> p m t", m=2)
        Vt = carve("Vt", NT * 258)
        V_ap = Vt[:, 0:NT * 258].rearrange("p (t e) -> p t e", e=258)
        bias_c = carve("bias_c", 1024, F32)
        cc_c = carve("cc_c", 128, F32)
        tmpf = [carve("tmpf%d" % i, 512, F32) for i in range(3)]
        ptd = [carve("ptd%d" % i, 512) for i in range(4)]
        accA = [carve("accA%d" % i, 256, F32) for i in range(4)]
        od = [carve("od%d" % i, 256, F32) for i in range(2)]
        odb = [carve("odb%d" % i, 256) for i in range(2)]
        rsd = [carve("rsd%d" % i, 4, F32) for i in range(2)]
        dstg = [carve("dstg%d" % i, 2 * 512) for i in range(2)]
        P.dma("sp", cc_c[:, 0:128], consts[:, C_CC:C_CC + 128], W=[cc_c])
        P.add("dve", f_memset(Vt[:, :], 1.0), W=[Vt])
        sc_d = 128 ** -0.5
        OB = (0, 1, 2, 3)
        SB_ = (4, 5, 6)
        TB_ = (7,)
        PB_ = (4, 5, 6, 7)
        for h in range(4):
            load_w(wd_c, wd_ap[:, :, 0:256], w_in[l], OFF_DQ + h * 256, 256)
            load_w(wd_c, wd_ap[:, :, 256:512], w_in[l], OFF_DK + h * 256, 256)
            load_w(wd_c, wd_ap[:, :, 512:768], w_in[l], OFF_DV + h * 256, 256)
            P.dma("sp", bias_c[:, 0:1024], consts[:, C_B + h * 1024:C_B + (h + 1) * 1024], W=[bias_c])
            for tb in range(NB):
                for c in range(4):
                    b = bank(PB_)
                    for kc in range(8):
                        a, tl = hT_blk(kc, tb)
                        P.add("pe", f_mm(b[:, 0:512], wd_ap[:, kc, c * 128:(c + 1) * 128], a, kc == 0, kc == 7), R=[wd_c] + tl, W=[b])
                    dst_t = dqT if c < 2 else dkT
                    dst_ap = (dqT_ap if c < 2 else dkT_ap)[:, c % 2, tb * 512:(tb + 1) * 512]
                    copy_on(evac_eng(), dst_ap, b[:, 0:512], [b], [dst_t])
            for t in range(NT):
                b = bank(PB_)
                for kc in range(8):
                    a, tl = hT_tok(kc, t)
                    P.add("pe", f_mm(b[:, 0:256], a, wd_ap[:, kc, 512:768], kc == 0, kc == 7), R=[wd_c] + tl, W=[b])
                copy_on(evac_eng(), V_ap[:, t, 0:256], b[:, 0:256], [b], [Vt])
            tiles = [(g, m, j) for g in range(NB) for m in range(2) for j in range(4 * g + 4)]
            LOOK = 2
            pend = {}

            def emit_qk(idx):
                g, m, j = tiles[idx]
                r = j - 4 * g
                q0c = max(r, 0) * 128
                nq = 512 - q0c
                b = bank(SB_)
                P.add("pe", f_mm(b[:, 0:nq], dkT_ap[:, m, j * 128:(j + 1) * 128], dqT_ap[:, m, g * 512 + q0c:(g + 1) * 512], True, True), R=[dkT, dqT], W=[b])
                tf = tmpf[idx % 3]
                pt = ptd[idx % 4]
                if r >= 0:
                    P.add("dve", f_stt(tf[:, 0:nq], b[:, 0:nq], sc_d, bias_c[:, 512:512 + nq], ALU.mult, ALU.add), R=[b, bias_c], W=[tf])
                    P.add("act", f_act(pt[:, 0:nq], tf[:, 0:nq], AF.Exp), R=[tf], W=[pt])
                else:
                    P.add("dve", f_stt(tf[:, 0:nq], b[:, 0:nq], sc_d, bias_c[:, 0:nq], ALU.mult, ALU.add), R=[b, bias_c], W=[tf])
                    dt_ = 4 * g - j
                    P.add("act", f_act(pt[:, 0:nq], tf[:, 0:nq], AF.Exp, bias=cc_c[:, h * 32 + dt_:h * 32 + dt_ + 1]), R=[tf, cc_c], W=[pt])
                pend[idx] = (pt, r, q0c)

            def emit_pv(idx):
                g, m, j = tiles[idx]
                pt, r, q0c = pend.pop(idx)
                for qt in range(max(r, 0), 4):
                    last_j = 4 * g + qt
                    P.add("pe", f_mm(obs[qt][:, 0:258], pt[:, qt * 128 - q0c:(qt + 1) * 128 - q0c], V_ap[:, j, :], j == 0, j == last_j), R=[pt, Vt], W=[obs[qt]])

            def finish_pass(g, m):
                dst_ = dstg[g % 2]
                dst_ap2 = dst_[:, :].rearrange("p (c t) -> p c t", c=2)
                for qt in range(4):
                    ob = obs[qt]
                    rs = rsd[qt % 2]
                    P.add("dve", lambda e, o=rs[:, 0:1], i=ob[:, 256:257]: e.reciprocal(out=o, in_=i), R=[ob], W=[rs])
                    if m == 0:
                        P.add("dve", f_ts(accA[qt][:, 0:256], ob[:, 0:256], rs[:, 0:1], None, ALU.mult), R=[ob, rs], W=[accA[qt]])
                    else:
                        P.add("dve", f_tt(rs[:, 1:2], rs[:, 0:1], lamt[:, 5:6], ALU.mult), R=[rs, lamt], W=[rs])
                        o_ = od[qt % 2]
                        P.add("dve", f_stt(o_[:, 0:256], ob[:, 0:256], rs[:, 1:2], accA[qt][:, 0:256], ALU.mult, ALU.add), R=[ob, rs, accA[qt]], W=[o_])
                        ss = sst[qt % 3]
                        P.add("act", f_act(junk[:, 0:256], o_[:, 0:256], AF.Square, accum_out=ss[:, 0:1]), R=[o_], W=[junk, ss])
                        rstd_from(ss, ss[:, 0:1], 256, [ss])
                        ob_ = odb[qt % 2]
                        P.add("dve", f_ts(ob_[:, 0:256], o_[:, 0:256], ss[:, 2:3], None, ALU.mult), R=[o_, ss], W=[ob_])
                        b2 = bank(TB_)
                        pb = psb(b2)
                        for c in range(2):
                            P.add("pe", f_tr(pb[:, c * 128:(c + 1) * 128], ob_[:, c * 128:(c + 1) * 128], ident[:]), R=[ob_, ident], W=[b2])
                        copy_on("act", dst_ap2[:, :, qt * 128:(qt + 1) * 128], pb[:, 0:256].rearrange("p (c t) -> p c t", c=2), [b2], [dst_])
                if m == 1:
                    dd = daT[h * 256:(h + 1) * 256, g * 512:(g + 1) * 512].rearrange("(c p) t -> p c t", p=128)
                    P.dma("sp", dd, dst_ap2, R=[dst_], W=[d_daT])

            obs = [ps[i] for i in OB]
            nt_ = len(tiles)
            for idx in range(min(LOOK, nt_)):
                emit_qk(idx)
            for idx in range(nt_):
                if idx + LOOK < nt_:
                    emit_qk(idx + LOOK)
                emit_pv(idx)
                g, m, j = tiles[idx]
                if j == 4 * g + 3:
                    finish_pass(g, m)
        _phase_end(5)

        o2[0] = 0
        wro = carve("wro", 16 * 1024)
        wdo = carve("wdo", 8 * 1024)
        wco = carve("wco", 8 * 1024)
        wou = carve("wou", 8 * 1024)
        wro_ap = wro[:, :].rearrange("p (c n) -> p c n", c=16)
        wdo_ap = wdo[:, :].rearrange("p (c n) -> p c n", c=8)
        wco_ap = wco[:, :].rearrange("p (c n) -> p c n", c=8)
        wou_ap = wou[:, :].rearrange("p (c n) -> p c n", c=8)
        load_w(wro, wro_ap[:, 0:8, :], w_ret_o[l][0:1024, :], 0, 1024)
        load_w(wro, wro_ap[:, 8:16, :], w_ret_o[l][1024:2048, :], 0, 1024)
        load_w(wdo, wdo_ap, w_diff_o[l], 0, 1024)
        load_w(wco, wco_ap, w_cross_o[l], 0, 1024)
        load_w(wou, wou_ap, w_out[l], 0, 1024)
        gcol = carve("gcol", 32, F32)
        P.dma("sp", gcol[:, 0:16], g_ret[l].rearrange("(c p) -> p c", p=128), W=[gcol], slow=True)
        P.dma("sp", gcol[:, 16:24], g_diff[l].rearrange("(c p) -> p c", p=128), W=[gcol], slow=True)
        P.add("dve", f_ts(gcol[:, 16:24], gcol[:, 16:24], 1.0 - lam_init, None, ALU.mult), R=[gcol], W=[gcol])
        for c in range(16):
            P.add("dve", f_ts(wro_ap[:, c, :], wro_ap[:, c, :], gcol[:, c:c + 1], None, ALU.mult), R=[wro, gcol], W=[wro])
        for c in range(8):
            P.add("dve", f_ts(wdo_ap[:, c, :], wdo_ap[:, c, :], gcol[:, 16 + c:17 + c], None, ALU.mult), R=[wdo, gcol], W=[wdo])
        inb = [carve("inb%d" % i, 16 * 512) for i in range(2)]
        inb2 = [carve("inc%d" % i, 16 * 512) for i in range(2)]
        gb_ = [carve("gb%d" % i, 3 * 512) for i in range(2)]
        mT = carve("mT", 8 * 512)
        mT_ap = mT[:, :].rearrange("p (c t) -> p c t", c=8)
        mf = [carve("mf%d" % i, 512, F32) for i in range(2)]
        mf2 = [carve("mg%d" % i, 512, F32) for i in range(2)]
        gi = 0
        for tb in range(NB):
            a_ = inb[tb % 2]
            c_ = inb2[tb % 2]
            a_ap = a_[:, :].rearrange("p (c t) -> p c t", c=16)
            c_ap = c_[:, :].rearrange("p (c t) -> p c t", c=16)
            P.dma("sp", a_ap, retT[:, tb * 512:(tb + 1) * 512].rearrange("(c p) t -> p c t", p=128), R=[d_retT], W=[a_])
            P.dma("sp", c_ap[:, 0:8, :], daT[:, tb * 512:(tb + 1) * 512].rearrange("(c p) t -> p c t", p=128), R=[d_daT], W=[c_])
            P.dma("sp", c_ap[:, 8:16, :], caT[:, tb * 512:(tb + 1) * 512].rearrange("(c p) t -> p c t", p=128), R=[d_caT], W=[c_])
            for cc in range(8):
                g_ = gb_[gi % 2]
                g_ap = g_[:, :].rearrange("p (b t) -> p b t", b=3)
                for br in range(3):
                    P.dma("sp", g_ap[:, br, :], gT[br * 1024 + cc * 128:br * 1024 + (cc + 1) * 128, tb * 512:(tb + 1) * 512], R=[d_gT], W=[g_])
                br_ = bank()
                for fc in range(16):
                    P.add("pe", f_mm(br_[:, 0:512], wro_ap[:, fc, cc * 128:(cc + 1) * 128], a_ap[:, fc, :], fc == 0, fc == 15), R=[wro, a_], W=[br_])
                bd_ = bank()
                for fc in range(8):
                    P.add("pe", f_mm(bd_[:, 0:512], wdo_ap[:, fc, cc * 128:(cc + 1) * 128], c_ap[:, fc, :], fc == 0, fc == 7), R=[wdo, c_], W=[bd_])
                bc_ = bank()
                for fc in range(8):
                    P.add("pe", f_mm(bc_[:, 0:512], wco_ap[:, fc, cc * 128:(cc + 1) * 128], c_ap[:, 8 + fc, :], fc == 0, fc == 7), R=[wco, c_], W=[bc_])
                m1, m2 = mf[gi % 2], mf2[gi % 2]
                gi += 1
                P.add("dve", f_tt(m1[:, 0:512], br_[:, 0:512], g_ap[:, 0, :], ALU.mult), R=[br_, g_], W=[m1])
                P.add("dve", f_tt(m2[:, 0:512], bd_[:, 0:512], g_ap[:, 1, :], ALU.mult), R=[bd_, g_], W=[m2])
                P.add("dve", f_tt(m1[:, 0:512], m1[:, 0:512], m2[:, 0:512], ALU.add), R=[m1, m2], W=[m1])
                P.add("dve", f_tt(m2[:, 0:512], bc_[:, 0:512], g_ap[:, 2, :], ALU.mult), R=[bc_, g_], W=[m2])
                P.add("dve", f_tt(mT_ap[:, cc, :], m1[:, 0:512], m2[:, 0:512], ALU.add), R=[m1, m2], W=[mT])
            for qt in range(4):
                t = tb * 4 + qt
                xt = xts[t % 3]
                P.dma("sp", xt[:], xsrc[t * 128:(t + 1) * 128, :], R=[d_xres], W=[xt])
                for half in range(2):
                    b = bank()
                    for cc in range(8):
                        P.add("pe", f_mm(b[:, 0:512], mT_ap[:, cc, qt * 128:(qt + 1) * 128], wou_ap[:, cc, half * 512:(half + 1) * 512], cc == 0, cc == 7), R=[mT, wou], W=[b])
                    P.add("dve", f_tt(xt[:, half * 512:(half + 1) * 512], b[:, 0:512], xt[:, half * 512:(half + 1) * 512], ALU.add), R=[b, xt], W=[xt])
                P.dma("sp", xres[t * 128:(t + 1) * 128, :], xt[:, 0:1024], R=[xt], W=[d_xres])
        _phase_end(6)

        o2[0] = 0
        wupq = [carve("wup%d" % q, 8192) for q in range(4)]
        wdnq = [carve("wdn%d" % q, 8192) for q in range(4)]
        wupq_ap = [w_[:, :].rearrange("p (c n) -> p c n", c=8) for w_ in wupq]
        wdnq_ap = [w_[:, :].rearrange("p (c n) -> p c n", c=8) for w_ in wdnq]
        for q in range(4):
            load_w(wupq[q], wupq_ap[q], w_up[l], q * 1024, 1024)
        for q in range(4):
            load_w(wdnq[q], wdnq_ap[q], w_down[l][q * 1024:(q + 1) * 1024, :], 0, 1024)
        h2T = [carve("h2T%d" % i, 8 * 512) for i in range(1)]
        uT = carve("uT", 32 * 512)
        uT_ap = uT[:, :].rearrange("p (c t) -> p c t", c=32)
        rl = [carve("rl%d" % i, 512) for i in range(3)]
        gfin = carve("gfin", 1024, F32)
        last = (l == NL - 1)
        P.dma("sp", gbc[:], g_ffn[l:l + 1, :].broadcast_to([128, D]), W=[gbc])
        if last:
            P.dma("sp", gfin[:, 0:1024], g_final[0:1, :].broadcast_to([128, D]), W=[gfin])
        ri = 0
        for tb in range(NB):
            h2 = h2T[0]
            h2_ap = h2[:, :].rearrange("p (c t) -> p c t", c=8)
            for qt in range(4):
                t = tb * 4 + qt
                xt, ss, hb = xts[t % 3], sst[t % 3], hbs[t % 2]
                P.dma("sp", xt[:], xres[t * 128:(t + 1) * 128, :], R=[d_xres], W=[xt])
                norm_tile(xt, ss, gbc, hb)
                transpose_to(hb, 8, h2_ap[:, :, qt * 128:(qt + 1) * 128], h2, eng="act")
            for fc in range(32):
                b = bank()
                for kc in range(8):
                    P.add("pe", f_mm(b[:, 0:512], wupq_ap[fc // 8][:, kc, (fc % 8) * 128:(fc % 8 + 1) * 128], h2_ap[:, kc, :], kc == 0, kc == 7), R=[wupq[fc // 8], h2], W=[b])
                r_ = rl[ri % 3]
                ri += 1
                P.add("dve", f_ts(r_[:, 0:512], b[:, 0:512], 0.0, None, ALU.max), R=[b], W=[r_])
                if fc % 2:
                    P.add("pool", f_tt(uT_ap[:, fc, :], r_[:, 0:512], r_[:, 0:512], ALU.mult), R=[r_], W=[uT])
                else:
                    P.add("act", f_act(uT_ap[:, fc, :], r_[:, 0:512], AF.Square), R=[r_], W=[uT])
            for qt in range(4):
                t = tb * 4 + qt
                xt = xts[t % 3]
                P.dma("sp", xt[:], xres[t * 128:(t + 1) * 128, :], R=[d_xres], W=[xt])
                for half in range(2):
                    b = bank()
                    for fc in range(32):
                        P.add("pe", f_mm(b[:, 0:512], uT_ap[:, fc, qt * 128:(qt + 1) * 128], wdnq_ap[fc // 8][:, fc % 8, half * 512:(half + 1) * 512], fc == 0, fc == 31), R=[uT, wdnq[fc // 8]], W=[b])
                    P.add("dve", f_tt(xt[:, half * 512:(half + 1) * 512], b[:, 0:512], xt[:, half * 512:(half + 1) * 512], ALU.add), R=[b, xt], W=[xt])
                if not last:
                    P.dma("sp", xres[t * 128:(t + 1) * 128, :], xt[:, 0:1024], R=[xt], W=[d_xres])
                else:
                    ss = sst[t % 3]
                    P.add("act", f_act(junk[:, 0:1024], xt[:, 0:1024], AF.Square, accum_out=ss[:, 0:1]), R=[xt], W=[junk, ss])
                    rstd_from(ss, ss[:, 0:1], 1024, [ss])
                    P.add("dve", f_stt(xt[:, 0:1024], xt[:, 0:1024], ss[:, 2:3], gfin[:, 0:1024], ALU.mult, ALU.mult), R=[xt, ss, gfin], W=[xt])
                    P.dma("sp", y_out[t * 128:(t + 1) * 128, :], xt[:, 0:1024], R=[xt], W=[d_y])
        _phase_end(7)

      except _Stop:
        break
    P.emit()
    es.close()
    return nc


_CACHE = {}


def kernel(**inputs):
    x = np.asarray(inputs["x"])
    B, S, _ = x.shape
    key = (S,)
    if key not in _CACHE:
        _CACHE[key] = build(S, 2)
    nc = _CACHE[key]
    consts = make_consts()
    shared = {k: np.ascontiguousarray(np.asarray(v)) for k, v in inputs.items() if k not in ("x", "mem")}
    shared["g_final"] = shared["g_final"].reshape(1, D)
    shared["consts"] = consts
    mem = np.asarray(inputs["mem"])
    in_maps = []
    for b in range(B):
        m = dict(shared)
        m["x"] = np.ascontiguousarray(x[b])
        m["mem"] = np.ascontiguousarray(mem[b])
        in_maps.append(m)
    res = run_bass_kernel_spmd(nc, in_maps, core_ids=list(range(B)))
    return np.stack([r["y"] for r in res.results], axis=0).astype(np.float32)
```

```python
import math
from contextlib import ExitStack

import numpy as np
import concourse.bass as bass
import concourse.mybir as mybir
from concourse.bass_utils import run_bass_kernel_spmd

F32 = mybir.dt.float32
BF16 = mybir.dt.bfloat16
AF = mybir.ActivationFunctionType
ALU = mybir.AluOpType
AX = mybir.AxisListType

D = 1024
NMEM = 256
DIN = 13312
DFF = 4096
OFF_RQ, OFF_RK, OFF_RV, OFF_RG = 0, 1024, 2048, 4096
OFF_DQ, OFF_DK, OFF_DV, OFF_CQ, OFF_G = 6144, 7168, 8192, 9216, 10240
EPS = 1e-6
NEG = -30000.0

C_ID = 0
C_B = 128
C_CC = C_B + 4 * 1024
C_RET = C_CC + 128
C_RET_W = 641
NCONST = C_RET + 4 * C_RET_W


def make_consts():
    c = np.zeros((128, NCONST), np.float64)
    c[:, C_ID:C_ID + 128] = np.eye(128)
    kk = np.arange(128)[:, None]
    qq = np.arange(512)[None, :]
    for h in range(4):
        slope = 2.0 ** (-8.0 * (h + 1) / 4)
        b = -slope * (qq - kk)
        c[:, C_B + h * 1024:C_B + h * 1024 + 512] = b
        c[:, C_B + h * 1024 + 512:C_B + h * 1024 + 1024] = np.where(qq >= kk, b, NEG)
        for dt in range(32):
            c[:, C_CC + h * 32 + dt] = -slope * dt * 128
    a = np.arange(128)[None, :]
    for h in range(4):
        lg = math.log(1.0 - 2.0 ** (-5.0 - h))
        o = C_RET + h * C_RET_W
        c[:, o:o + 128] = np.where(a >= kk, np.exp(-lg * (kk + 1.0)) * 256 ** -0.5, 0.0)
        for rep in range(4):
            c[:, o + 128 + rep * 128:o + 256 + rep * 128] = np.exp(lg * (a + 1.0)) * np.ones((128, 1))
        c[:, o + 640] = np.exp(lg * (127.0 - kk[:, 0])) * 256 ** -0.5
    return c.astype(np.float32)


class Tl:
    __slots__ = ("name", "ap", "w", "r", "dsem", "dtot", "acc")

    def __init__(self, name, ap=None, acc=False):
        self.name = name
        self.ap = ap
        self.w = {}
        self.r = {}
        self.dsem = None
        self.dtot = 0
        self.acc = acc

    def __getitem__(self, k):
        return self.ap[k]


ENGS = ("sp", "act", "dve", "pool", "pe")


class Prog:
    def __init__(self, nc, es):
        self.nc = nc
        self.es = es
        self.streams = {e: [] for e in ENGS}
        self.tiles = []
        self.esem = {e: es.enter_context(nc.semaphore("es_" + e)) for e in ENGS}
        self.dsems = []
        self.semh = []
        self.semtot = []
        self.semfree = []
        self.semeng = []
        self.lastc = {e: -1 for e in ENGS}

    def tile(self, name, ap=None, acc=False):
        t = Tl(name, ap, acc)
        self.tiles.append(t)
        return t

    def sb(self, name, shape, dt):
        h = self.es.enter_context(self.nc.sbuf_tensor(name, list(shape), dt))
        return self.tile(name, h[:] if len(shape) == 2 else h[tuple(slice(None) for _ in shape)])

    @staticmethod
    def _merge(d, s):
        for k, v in s.items():
            if d.get(k, -1) < v:
                d[k] = v

    def _deps(self, eng, R, W, is_dma):
        raw, oth = {}, {}
        for t in R:
            self._merge(raw, t.w)
        for t in W:
            if not t.acc:
                self._merge(oth, t.w)
            self._merge(oth, t.r)
        own = ("c", eng)
        if not is_dma and eng == "pe":
            oth.pop(own, None)
            raw.pop(own, None)
        self._merge(raw, oth)
        return raw

    def add(self, eng, fn, R=(), W=()):
        deps = self._deps(eng, R, W, False)
        idx = len(self.streams[eng])
        self.streams[eng].append([fn, deps, None])
        self.lastc[eng] = idx
        key = ("c", eng)
        for t in R:
            if t.r.get(key, -1) < idx:
                t.r[key] = idx
        for t in W:
            t.w = {key: idx}
            t.r = {}
        return idx

    def dma(self, eng, out, in_, R=(), W=(), st=None, slow=False):
        if st is None:
            st = [t for t in list(W) + list(R) if t.ap is not None][0]
        if st.dsem is not None and self.semeng[st.dsem] != eng:
            raise RuntimeError("tile %s used by DMAs of two queues" % st.name)
        if st.dsem is None:
            fl = [i for i in self.semfree if self.semeng[i] == eng]
            if fl:
                st.dsem = fl[-1]
                self.semfree.remove(st.dsem)
            else:
                st.dsem = len(self.semh)
                self.semh.append(self.es.enter_context(self.nc.semaphore("ds%d" % st.dsem)))
                self.semtot.append(0)
                self.semeng.append(eng)
            self.dsems.append(st)
        deps = self._deps(eng, R, W, True)
        si = st.dsem
        self.semtot[si] += 16
        tot = self.semtot[si]
        key = ("d", si)
        if slow:
            fn = lambda e, o=out, i=in_: e.dma_start(out=o, in_=i, allow_slow_non_contiguous=True)
        else:
            fn = lambda e, o=out, i=in_: e.dma_start(out=o, in_=i)
        self.streams[eng].append([fn, deps, (self.semh[si], 16)])
        for t in R:
            t.r[key] = tot
        for t in W:
            if t.acc:
                t.w[key] = tot
            else:
                t.w = {key: tot}
                t.r = {}

    def barrier(self):
        last = dict(self.lastc)
        dd = {("d", i): v for i, v in enumerate(self.semtot) if v > 0}
        for e in ENGS:
            deps = dict(dd)
            for e2 in ENGS:
                if e2 != e and last[e2] >= 0:
                    deps[("c", e2)] = last[e2]
            self.streams[e].append([None, deps, None])
        for t in self.tiles:
            t.w = {}
            t.r = {}
        for t in self.dsems:
            t.dsem = None
        self.dsems = []
        self.semfree = list(range(len(self.semh)))

    def emit(self):
        nc = self.nc
        need = {e: set() for e in ENGS}
        for e in ENGS:
            for fn, deps, _ in self.streams[e]:
                for k, v in deps.items():
                    if k[0] == "c":
                        need[k[1]].add(v)
        cnt = {}
        for e in ENGS:
            c = 0
            arr = []
            for i in range(len(self.streams[e])):
                if i in need[e]:
                    c += 1
                arr.append(c)
            cnt[e] = arr
        engobj = {"sp": nc.sync, "act": nc.scalar, "dve": nc.vector, "pool": nc.gpsimd, "pe": nc.tensor}
        semof = self.semh

        def run(e):
            def body(eng):
                waited = {}
                for i, (fn, deps, dinc) in enumerate(self.streams[e]):
                    for k, v in deps.items():
                        if k[0] == "c":
                            sem, val = self.esem[k[1]], cnt[k[1]][v]
                        else:
                            sem, val = semof[k[1]], v
                        sk = id(sem)
                        if waited.get(sk, 0) < val:
                            eng.wait_ge(sem, val)
                            waited[sk] = val
                    if fn is None:
                        if i in need[e]:
                            eng.nop().then_inc(self.esem[e], 1)
                        continue
                    ins = fn(eng)
                    if dinc is not None:
                        ins.then_inc(dinc[0], dinc[1])
                    elif i in need[e]:
                        ins.then_inc(self.esem[e], 1)
            return body

        with nc.Block() as block:
            block.sync(run("sp"))
            block.scalar(run("act"))
            block.vector(run("dve"))
            block.gpsimd(run("pool"))
            block.tensor(run("pe"))


def f_mm(out, lhsT, rhs, start, stop):
    return lambda e: e.matmul(out, lhsT, rhs, start=start, stop=stop)


def f_tr(out, in_, ident):
    return lambda e: e.transpose(out=out, in_=in_, identity=ident)


def f_act(out, in_, func, **kw):
    return lambda e: e.activation(out=out, in_=in_, func=func, **kw)


def f_copy(out, in_):
    return lambda e: e.tensor_copy(out, in_)


def f_tt(out, a, b, op):
    return lambda e: e.tensor_tensor(out=out, in0=a, in1=b, op=op)


def f_ts(out, a, s1, s2, op0, op1=None):
    if op1 is None:
        return lambda e: e.tensor_scalar(out=out, in0=a, scalar1=s1, scalar2=None, op0=op0)
    return lambda e: e.tensor_scalar(out=out, in0=a, scalar1=s1, scalar2=s2, op0=op0, op1=op1)


def f_stt(out, a, s, b, op0, op1):
    return lambda e: e.scalar_tensor_tensor(out=out, in0=a, scalar=s, in1=b, op0=op0, op1=op1)


def f_memset(ap, v):
    return lambda e: e.memset(ap, v)


class _Stop(Exception):
    pass


def build(S, NL, dbg=False, stop=None):
    NT = S // 128
    NB = S // 512
    nc = bass.Bass("TRN2", target_bir_lowering=False)
    es = ExitStack()
    P = Prog(nc, es)

    def din(name, shape, dt=F32):
        return nc.dram_tensor(name, list(shape), dt, kind="ExternalInput").ap()

    x_in = din("x", [S, D])
    mem_in = din("mem", [NMEM, D])
    g_mix = din("g_mix", [2, D])
    w_in = din("w_in", [2, D, DIN])
    g_ret = din("g_ret", [2, 2048])
    w_ret_o = din("w_ret_o", [2, 2048, D])
    lq1 = din("lambda_q1", [2, 128])
    lk1 = din("lambda_k1", [2, 128])
    lq2 = din("lambda_q2", [2, 128])
    lk2 = din("lambda_k2", [2, 128])
    g_diff = din("g_diff", [2, D])
    w_diff_o = din("w_diff_o", [2, D, D])
    g_mem = din("g_mem", [2, D])
    w_mem_kv = din("w_mem_kv", [2, D, 2048])
    w_cross_o = din("w_cross_o", [2, D, D])
    w_out = din("w_out", [2, D, D])
    g_ffn = din("g_ffn", [2, D])
    w_up = din("w_up", [2, D, DFF])
    w_down = din("w_down", [2, DFF, D])
    g_final = din("g_final", [1, D])
    consts = din("consts", [128, NCONST])
    y_out = nc.dram_tensor("y", [S, D], F32, kind="ExternalOutput").ap()

    kind_s = "ExternalOutput" if dbg else "Internal"
    xres = nc.dram_tensor("xres", [S, D], F32, kind=kind_s).ap()
    retT = nc.dram_tensor("retT", [2048, S], BF16, kind=kind_s).ap()
    daT = nc.dram_tensor("daT", [1024, S], BF16, kind=kind_s).ap()
    caT = nc.dram_tensor("caT", [1024, S], BF16, kind=kind_s).ap()
    gT = nc.dram_tensor("gT", [3072, S], BF16, kind=kind_s).ap()
    d_xres = P.tile("d_xres", acc=True)
    d_retT = P.tile("d_retT", acc=True)
    d_daT = P.tile("d_daT", acc=True)
    d_caT = P.tile("d_caT", acc=True)
    d_gT = P.tile("d_gT", acc=True)
    d_y = P.tile("d_y", acc=True)

    ps = []
    for i in range(8):
        h = es.enter_context(nc.psum_tensor("ps%d" % i, [128, 512], F32))
        ps.append(P.tile("ps%d" % i, h[:]))
    ALLB = list(range(8))
    rr = {}

    def bank(pool=None):
        pool = tuple(pool or ALLB)
        i = rr.get(pool, -1) + 1
        rr[pool] = i
        return ps[pool[i % len(pool)]]

    def psb(t):
        return t.ap.bitcast(BF16)

    identf = P.sb("identf", [128, 128], F32)
    ident = P.sb("ident", [128, 128], BF16)
    epst = P.sb("epst", [128, 1], F32)
    lamt = P.sb("lamt", [128, 8], F32)
    junk = P.sb("junk", [128, 1024], BF16)
    gbc = P.sb("gbc", [128, 1024], F32)
    xts = [P.sb("xt%d" % i, [128, 1024], F32) for i in range(3)]
    sst = [P.sb("ss%d" % i, [128, 4], F32) for i in range(3)]
    hbs = [P.sb("hb%d" % i, [128, 1024], BF16) for i in range(2)]
    NAR = 93696
    ARh = es.enter_context(nc.sbuf_tensor("arena", [128, NAR], BF16))
    AR = ARh[:]
    o2 = [0]

    def carve(name, n, dt=BF16):
        if dt == F32:
            a = AR[:, o2[0]:o2[0] + 2 * n].bitcast(F32)
            o2[0] += 2 * n
        else:
            a = AR[:, o2[0]:o2[0] + n]
            o2[0] += n + (n & 1)
        assert o2[0] <= NAR - 4096, (name, o2[0])
        return P.tile(name, a)

    evac_rr = [0]

    def evac_eng():
        evac_rr[0] ^= 1
        return "act" if evac_rr[0] else "dve"

    def copy_on(eng, out, in_, R, W):
        if eng == "act":
            P.add("act", f_act(out, in_, AF.Copy), R=R, W=W)
        else:
            P.add(eng, f_copy(out, in_), R=R, W=W)

    P.dma("sp", identf[:], consts[:, C_ID:C_ID + 128], W=[identf])
    P.add("dve", f_copy(ident[:], identf[:]), R=[identf], W=[ident])
    P.add("dve", f_memset(epst[:], EPS), W=[epst])

    def rstd_from(ss, src_ap, n, R):
        P.add("act", f_act(ss[:, 1:2], src_ap, AF.Ln, scale=1.0 / n, bias=epst[:, 0:1]), R=R + [epst], W=[ss])
        P.add("act", f_act(ss[:, 2:3], ss[:, 1:2], AF.Exp, scale=-0.5), R=[ss], W=[ss])

    def norm_tile(xt, ss, gb, hb, n=1024):
        P.add("act", f_act(junk[:, 0:n], xt[:, 0:n], AF.Square, accum_out=ss[:, 0:1]), R=[xt], W=[junk, ss])
        rstd_from(ss, ss[:, 0:1], n, [ss])
        P.add("dve", f_stt(hb[:, 0:n], xt[:, 0:n], ss[:, 2:3], gb[:, 0:n], ALU.mult, ALU.mult), R=[xt, ss, gb], W=[hb])

    def transpose_to(hb, nchunk, dst_ap, dst_tile, eng=None, pool=None):
        b = bank(pool)
        pb = psb(b)
        for c in range(nchunk):
            P.add("pe", f_tr(pb[:, c * 128:(c + 1) * 128], hb[:, c * 128:(c + 1) * 128], ident[:]), R=[hb, ident], W=[b])
        copy_on(eng or evac_eng(), dst_ap, pb[:, 0:nchunk * 128].rearrange("p (c t) -> p c t", c=nchunk), [b], [dst_tile])

    wstage = [P.tile("wstage%d" % i, AR[:, NAR - 4096 + i * 2048:NAR - 4096 + (i + 1) * 2048].bitcast(F32)) for i in range(2)]
    wsi = [0]

    def load_w(tile, dst_ap, w_l, c0, n, defer=False):
        src = w_l.rearrange("(kc p) n -> p kc n", p=128)
        if n >= 1024:
            pieces = [(kc, 1, cs, 1024) for kc in range(8) for cs in range(0, n, 1024)]
        else:
            kcn = 1024 // n
            pieces = [(kc, kcn, 0, n) for kc in range(0, 8, kcn)]
        def piece(kc0, kcn, cs, ncol):
            st = wstage[wsi[0] % 2]
            wsi[0] += 1
            st_ap = st[:, 0:kcn * ncol].rearrange("p (k n) -> p k n", k=kcn)
            P.dma("sp", st_ap, src[:, kc0:kc0 + kcn, c0 + cs:c0 + cs + ncol], W=[st])
            copy_on(evac_eng(), dst_ap[:, kc0:kc0 + kcn, cs:cs + ncol], st_ap, [st], [tile])
        if defer:
            return [lambda a=a: piece(*a) for a in pieces]
        for a in pieces:
            piece(*a)

    def _phase_end(k):
        P.barrier()
        if stop == k:
            raise _Stop()

    for l in range(NL):
      try:
        xsrc = x_in if l == 0 else xres
        lam_init = 0.8 - 0.6 * math.exp(-0.3 * l)
        hT_ap = AR[:, 0:8 * S].rearrange("p (c t) -> p c t", c=8)
        hTt = [P.tile("hT%d" % t) for t in range(NT)]
        P.dma("sp", gbc[:], g_mix[l:l + 1, :].broadcast_to([128, D]), W=[gbc])
        for t in range(NT):
            xt, ss, hb = xts[t % 3], sst[t % 3], hbs[t % 2]
            P.dma("sp", xt[:], xsrc[t * 128:(t + 1) * 128, :], R=[d_xres], W=[xt])
            norm_tile(xt, ss, gbc, hb)
            transpose_to(hb, 8, hT_ap[:, :, t * 128:(t + 1) * 128], hTt[t])

        def hT_blk(kc, tb):
            return hT_ap[:, kc, tb * 512:(tb + 1) * 512], hTt[tb * 4:(tb + 1) * 4]

        def hT_tok(kc, t):
            return hT_ap[:, kc, t * 128:(t + 1) * 128], [hTt[t]]

        o2[0] = 8 * S
        lt = carve("lt", 512, F32)
        lt2 = carve("lt2", 256, F32)
        for i, srcl in enumerate((lq1, lk1, lq2, lk2)):
            P.dma("sp", lt[:, i * 128:(i + 1) * 128], srcl[l:l + 1, :].broadcast_to([128, 128]), W=[lt])
        P.add("dve", f_tt(lt2[:, 0:128], lt[:, 0:128], lt[:, 128:256], ALU.mult), R=[lt], W=[lt2])
        P.add("dve", f_tt(lt2[:, 128:256], lt[:, 256:384], lt[:, 384:512], ALU.mult), R=[lt], W=[lt2])
        P.add("dve", lambda e: e.reduce_sum(out=lamt[:, 0:2], in_=lt2[:, 0:256].rearrange("p (a b) -> p a b", a=2), axis=AX.X), R=[lt2], W=[lamt])
        P.add("act", f_act(lamt[:, 2:4], lamt[:, 0:2], AF.Exp), R=[lamt], W=[lamt])
        P.add("dve", f_tt(lamt[:, 4:5], lamt[:, 2:3], lamt[:, 3:4], ALU.subtract), R=[lamt], W=[lamt])
        P.add("dve", f_ts(lamt[:, 5:6], lamt[:, 4:5], -1.0, -lam_init, ALU.mult, ALU.add), R=[lamt], W=[lamt])
        _phase_end(0)

        o2[0] = 8 * S
        mkT = carve("mkT", 8 * 256)
        mkT_ap = mkT[:, :].rearrange("p (c t) -> p c t", c=8)
        mv = carve("mv", 2 * 4 * 258)
        mv_ap = mv[:, :].rearrange("p (m h e) -> p m h e", m=2, h=4)
        o_mix = o2[0]
        mhT = carve("mhT", 8 * 256)
        mhT_ap = mhT[:, :].rearrange("p (c t) -> p c t", c=8)
        wkv = carve("wkv", 8 * 1024)
        wkv_ap = wkv[:, :].rearrange("p (c n) -> p c n", c=8)
        P.dma("sp", gbc[:], g_mem[l:l + 1, :].broadcast_to([128, D]), W=[gbc])
        for t in range(2):
            xt, ss, hb = xts[t % 3], sst[t % 3], hbs[t % 2]
            P.dma("sp", xt[:], mem_in[t * 128:(t + 1) * 128, :], W=[xt])
            norm_tile(xt, ss, gbc, hb)
            transpose_to(hb, 8, mhT_ap[:, :, t * 128:(t + 1) * 128], mhT, eng="act")
        if stop == 0.5:
            _phase_end(0.5)
        P.add("dve", f_memset(mv[:, :], 1.0), W=[mv])
        load_w(wkv, wkv_ap, w_mem_kv[l], 0, 1024)
        if stop == 0.6:
            _phase_end(0.6)
        for cc in range(8):
            b = bank()
            for kc in range(8):
                P.add("pe", f_mm(b[:, 0:256], wkv_ap[:, kc, cc * 128:(cc + 1) * 128], mhT_ap[:, kc, :], kc == 0, kc == 7), R=[wkv, mhT], W=[b])
            copy_on("act", mkT_ap[:, cc, :], b[:, 0:256], [b], [mkT])
        if stop == 0.7:
            _phase_end(0.7)
        load_w(wkv, wkv_ap, w_mem_kv[l], 1024, 1024)
        for mt in range(2):
            for half in range(2):
                b = bank()
                for kc in range(8):
                    P.add("pe", f_mm(b[:, 0:512], mhT_ap[:, kc, mt * 128:(mt + 1) * 128], wkv_ap[:, kc, half * 512:(half + 1) * 512], kc == 0, kc == 7), R=[wkv, mhT], W=[b])
                copy_on("dve", mv_ap[:, mt, 2 * half:2 * half + 2, 0:256], b[:, 0:512].rearrange("p (h e) -> p h e", h=2), [b], [mv])
        _phase_end(1)

        o2[0] = o_mix
        wq_c = [carve("wq_c%d" % i, 8 * 256) for i in range(2)]
        cqT = carve("cqT", 2 * S)
        cqT_ap = cqT[:, :].rearrange("p (c t) -> p c t", c=2)
        pts = [carve("pt%d" % i, 512) for i in range(4)]
        oq = [carve("oq%d" % i, 256) for i in range(2)]
        rsm = [carve("rsm%d" % i, 4, F32) for i in range(4)]
        stg = [carve("stg%d" % i, 2 * 512) for i in range(2)]
        sc_c = 256 ** -0.5
        for h in range(4):
            wq = wq_c[h % 2]
            wq_ap = wq[:, :].rearrange("p (c n) -> p c n", c=8)
            load_w(wq, wq_ap, w_in[l], OFF_CQ + h * 256, 256)
            for tb in range(NB):
                for c in range(2):
                    b = bank()
                    for kc in range(8):
                        a, tl = hT_blk(kc, tb)
                        P.add("pe", f_mm(b[:, 0:512], wq_ap[:, kc, c * 128:(c + 1) * 128], a, kc == 0, kc == 7), R=[wq] + tl, W=[b])
                    copy_on(evac_eng(), cqT_ap[:, c, tb * 512:(tb + 1) * 512], b[:, 0:512], [b], [cqT])
            for qb in range(NB):
                st_ = stg[qb % 2]
                st_ap = st_[:, :].rearrange("p (c t) -> p c t", c=2)
                pt2 = []
                for mt in range(2):
                    b = bank()
                    for c in range(2):
                        P.add("pe", f_mm(b[:, 0:512], mkT_ap[:, h * 2 + c, mt * 128:(mt + 1) * 128], cqT_ap[:, c, qb * 512:(qb + 1) * 512], c == 0, c == 1), R=[mkT, cqT], W=[b])
                    pt = pts[(qb * 2 + mt) % 4]
                    P.add("act", f_act(pt[:, 0:512], b[:, 0:512], AF.Exp, scale=sc_c), R=[b], W=[pt])
                    pt2.append(pt)
                for qt in range(4):
                    b = bank()
                    for mt in range(2):
                        P.add("pe", f_mm(b[:, 0:258], pt2[mt][:, qt * 128:(qt + 1) * 128], mv_ap[:, mt, h, :], mt == 0, mt == 1), R=[pt2[mt], mv], W=[b])
                    rs = rsm[qt]
                    P.add("dve", lambda e, o=rs[:, 0:1], i=b[:, 256:257]: e.reciprocal(out=o, in_=i), R=[b], W=[rs])
                    o_ = oq[qt % 2]
                    P.add("dve", f_ts(o_[:, 0:256], b[:, 0:256], rs[:, 0:1], None, ALU.mult), R=[b, rs], W=[o_])
                    b2 = bank()
                    pb = psb(b2)
                    for c in range(2):
                        P.add("pe", f_tr(pb[:, c * 128:(c + 1) * 128], o_[:, c * 128:(c + 1) * 128], ident[:]), R=[o_, ident], W=[b2])
                    copy_on("act", st_ap[:, :, qt * 128:(qt + 1) * 128], pb[:, 0:256].rearrange("p (c t) -> p c t", c=2), [b2], [st_])
                dst = caT[h * 256:(h + 1) * 256, qb * 512:(qb + 1) * 512].rearrange("(c p) t -> p c t", p=128)
                P.dma("sp", dst, st_ap, R=[st_], W=[d_caT])
        _phase_end(2)

        o2[0] = o_mix
        wg_c = [carve("wg%d" % i, 8 * 512) for i in range(2)]
        gst = [carve("gst%d" % i, 2048) for i in range(2)]
        gex = [carve("gex%d" % i, 512, F32) for i in range(2)]
        gxi = [0]
        gi = 0
        for grp in range(6):
            wg = wg_c[grp % 2]
            wg_ap = wg[:, :].rearrange("p (c n) -> p c n", c=8)
            load_w(wg, wg_ap, w_in[l], OFF_G + grp * 512, 512)
            for cc in range(4):
                for tb4 in range((NB + 3) // 4):
                    g_ = gst[gi % 2]
                    gi += 1
                    nb_here = min(4, NB - tb4 * 4)
                    for tbi in range(nb_here):
                        tb = tb4 * 4 + tbi
                        b = bank()
                        for kc in range(8):
                            a, tl = hT_blk(kc, tb)
                            P.add("pe", f_mm(b[:, 0:512], wg_ap[:, kc, cc * 128:(cc + 1) * 128], a, kc == 0, kc == 7), R=[wg] + tl, W=[b])
                        ge = gex[gxi[0] % 2]
                        gxi[0] += 1
                        P.add("act", f_act(ge[:, 0:512], b[:, 0:512], AF.Exp, scale=-1.0), R=[b], W=[ge])
                        P.add("act", f_act(ge[:, 0:512], ge[:, 0:512], AF.Ln, bias=1.0), R=[ge], W=[ge])
                        P.add("act", f_act(g_[:, tbi * 512:(tbi + 1) * 512], ge[:, 0:512], AF.Exp, scale=-1.0), R=[ge], W=[g_])
                    row = grp * 512 + cc * 128
                    P.dma("sp", gT[row:row + 128, tb4 * 2048:tb4 * 2048 + nb_here * 512], g_[:, 0:nb_here * 512], R=[g_], W=[d_gT])
        _phase_end(3)
        if stop == 3.5:
            _phase_end(3.5)

        o2[0] = o_mix
        wr_cs = [carve("wr%d" % i, 8 * 1536) for i in range(2)]
        wr_aps = [w_[:, :].rearrange("p (c n) -> p c n", c=8) for w_ in wr_cs]

        def wr_loads(hh, defer):
            w_, a_ = wr_cs[hh % 2], wr_aps[hh % 2]
            out = []
            for (lo, hi, off, n_) in ((0, 256, OFF_RQ + hh * 256, 256), (256, 512, OFF_RK + hh * 256, 256),
                                      (512, 1024, OFF_RV + hh * 512, 512), (1024, 1536, OFF_RG + hh * 512, 512)):
                r_ = load_w(w_, a_[:, :, lo:hi], w_in[l], off, n_, defer=defer)
                if defer:
                    out += r_
            return out
        rc = carve("rc", C_RET_W + 1, F32)
        qk_blk = [[carve("qk%d_%d" % (i, c), 512) for c in range(4)] for i in range(2)]
        kd_sb = [carve("kd_sb%d" % i, 256) for i in range(2)]
        v_sb = [carve("v_sb%d" % i, 512) for i in range(2)]
        sg_sb = [carve("sg_sb%d" % i, 512) for i in range(2)]
        xs_sb = [carve("xs_sb%d" % i, 512, F32) for i in range(2)]
        ex_sb = [carve("ex_sb%d" % i, 512, F32) for i in range(2)]
        pT_sb = [carve("pT_sb%d" % i, 128) for i in range(2)]
        Sf = [carve("Sf%d" % i, 512, F32) for i in range(2)]
        Sb = [carve("Sb%d" % i, 512) for i in range(2)]
        yn = [carve("yn%d" % i, 512, F32) for i in range(2)]
        yg = [carve("yg%d" % i, 512) for i in range(2)]
        bst = [carve("bst%d" % i, 8, F32) for i in range(2)]
        rstg = [carve("rstg%d" % i, 4 * 512) for i in range(2)]
        for h in range(4):
            lg = math.log(1.0 - 2.0 ** (-5.0 - h))
            chd = math.exp(lg * 128.0)
            wr_c, wr_ap = wr_cs[h % 2], wr_aps[h % 2]
            if h == 0:
                wr_loads(0, False)
            drip = wr_loads(h + 1, True) if h + 1 < 4 else []
            P.dma("sp", rc[:, 0:C_RET_W], consts[:, C_RET + h * C_RET_W:C_RET + (h + 1) * C_RET_W], W=[rc])
            M_ap = rc[:, 0:128]
            R4_ap = rc[:, 128:640]
            sd_ap = rc[:, 640:641]

            def qk_proj(tb):
                for c in range(4):
                    b = bank()
                    for kc in range(8):
                        a, tl = hT_blk(kc, tb)
                        P.add("pe", f_mm(b[:, 0:512], wr_ap[:, kc, c * 128:(c + 1) * 128], a, kc == 0, kc == 7), R=[wr_c] + tl, W=[b])
                    dst = qk_blk[tb % 2][c]
                    if c < 2:
                        P.add("dve", f_tt(dst[:, 0:512], b[:, 0:512], R4_ap, ALU.mult), R=[b, rc], W=[dst])
                    else:
                        copy_on("act", dst[:, 0:512], b[:, 0:512], [b], [dst])

            def ret_proj(t):
                i = t % 2
                if stop == 3.56:
                    _phase_end(3.56)
                b = bank()
                for kc in range(8):
                    a, tl = hT_tok(kc, t)
                    P.add("pe", f_mm(b[:, 0:256], a, wr_ap[:, kc, 256:512], kc == 0, kc == 7), R=[wr_c] + tl, W=[b])
                P.add("dve", f_ts(kd_sb[i][:, 0:256], b[:, 0:256], sd_ap, None, ALU.mult), R=[b, rc], W=[kd_sb[i]])
                if stop == 3.57:
                    _phase_end(3.57)
                b = bank()
                for kc in range(8):
                    a, tl = hT_tok(kc, t)
                    P.add("pe", f_mm(b[:, 0:512], a, wr_ap[:, kc, 512:1024], kc == 0, kc == 7), R=[wr_c] + tl, W=[b])
                copy_on("dve", v_sb[i][:, 0:512], b[:, 0:512], [b], [v_sb[i]])
                if stop == 3.58:
                    _phase_end(3.58)
                b = bank()
                for kc in range(8):
                    a, tl = hT_tok(kc, t)
                    P.add("pe", f_mm(b[:, 0:512], a, wr_ap[:, kc, 1024:1536], kc == 0, kc == 7), R=[wr_c] + tl, W=[b])
                P.add("act", f_act(ex_sb[i][:, 0:512], b[:, 0:512], AF.Exp, scale=-1.0), R=[b], W=[ex_sb[i]])
                P.add("act", f_act(ex_sb[i][:, 0:512], ex_sb[i][:, 0:512], AF.Ln, bias=1.0), R=[ex_sb[i]], W=[ex_sb[i]])
                P.add("act", f_act(ex_sb[i][:, 0:512], ex_sb[i][:, 0:512], AF.Exp, scale=-1.0), R=[ex_sb[i]], W=[ex_sb[i]])
                P.add("dve", f_tt(sg_sb[i][:, 0:512], b[:, 0:512], ex_sb[i][:, 0:512], ALU.mult), R=[b, ex_sb[i]], W=[sg_sb[i]])

            if stop == 3.55:
                _phase_end(3.55)
            qk_proj(0)
            ret_proj(0)
            if stop == 3.6:
                _phase_end(3.6)
            for t in range(NT):
                i = t % 2
                qb_ = qk_blk[(t // 4) % 2]
                tsl = slice((t % 4) * 128, (t % 4 + 1) * 128)
                b = bank()
                for c in range(2):
                    P.add("pe", f_mm(b[:, 0:128], qb_[2 + c][:, tsl], qb_[c][:, tsl], c == 0, c == 1), R=[qb_[2 + c], qb_[c]], W=[b])
                P.add("dve", f_tt(pT_sb[i][:, 0:128], b[:, 0:128], M_ap, ALU.mult), R=[b, rc], W=[pT_sb[i]])
                if t + 1 < NT:
                    ret_proj(t + 1)
                if t % 4 == 1 and t // 4 + 1 < NB:
                    qk_proj(t // 4 + 1)
                ndrip = (len(drip) + max(NT - 2 - t, 1) - 1) // max(NT - 2 - t, 1) if drip else 0
                for _ in range(ndrip):
                    drip.pop(0)()
                bo = bank()
                P.add("pe", f_mm(bo[:, 0:512], pT_sb[i][:, 0:128], v_sb[i][:, 0:512], True, t == 0), R=[pT_sb[i], v_sb[i]], W=[bo])
                if t > 0:
                    for c in range(2):
                        P.add("pe", f_mm(bo[:, 0:512], qb_[c][:, tsl], Sb[c][:, 0:512], False, c == 1), R=[qb_[c], Sb[c]], W=[bo])
                if t + 1 < NT:
                    for c in range(2):
                        bs = bank()
                        P.add("pe", f_mm(bs[:, 0:512], kd_sb[i][:, c * 128:(c + 1) * 128], v_sb[i][:, 0:512], True, True), R=[kd_sb[i], v_sb[i]], W=[bs])
                        if t == 0:
                            copy_on("dve", Sf[c][:, 0:512], bs[:, 0:512], [bs], [Sf[c]])
                        else:
                            P.add("dve", f_stt(Sf[c][:, 0:512], Sf[c][:, 0:512], chd, bs[:, 0:512], ALU.mult, ALU.add), R=[bs, Sf[c]], W=[Sf[c]])
                        copy_on("act", Sb[c][:, 0:512], Sf[c][:, 0:512], [Sf[c]], [Sb[c]])
                if stop == 3.7:
                    _phase_end(3.7)
                st6 = bst[i]
                P.add("dve", lambda e, o=st6[:, 0:6], a=bo[:, 0:512]: e.bn_stats(out=o, in_=a), R=[bo], W=[st6])
                P.add("dve", lambda e, o=st6[:, 6:8], a=st6[:, 0:6]: e.bn_aggr(out=o, in_=a), R=[st6], W=[st6])
                ss = sst[t % 3]
                rstd_from(ss, st6[:, 7:8], 1, [st6])
                P.add("dve", f_ts(yn[i][:, 0:512], bo[:, 0:512], st6[:, 6:7], ss[:, 2:3], ALU.subtract, ALU.mult), R=[bo, st6, ss], W=[yn[i]])
                P.add("dve", f_tt(yg[i][:, 0:512], yn[i][:, 0:512], sg_sb[i][:, 0:512], ALU.mult), R=[yn[i], sg_sb[i]], W=[yg[i]])
                if stop == 3.8:
                    _phase_end(3.8)
                rs_ = rstg[(t // 4) % 2]
                rs_ap = rs_[:, :].rearrange("p (c t) -> p c t", c=4)
                transpose_to(yg[i], 4, rs_ap[:, :, (t % 4) * 128:(t % 4 + 1) * 128], rs_, eng="act")
                if t % 4 == 3:
                    tb = t // 4
                    dst = retT[h * 512:(h + 1) * 512, tb * 512:(tb + 1) * 512].rearrange("(c p) t -> p c t", p=128)
                    P.dma("sp", dst, rs_ap, R=[rs_], W=[d_retT])
        _phase_end(4)

        o2[0] = o_mix
        wd_c = carve("wd", 8 * 768)
        wd_ap = wd_c[:, :].rearrange("p (c n) -> p c n", c=8)
        dqT = carve("dqT", 2 * S)
        dkT = carve("dkT", 2 * S)
        dqT_ap = dqT[:, :].rearrange("p (m t) -> p m t", m=2)
        dkT_ap = dkT[:, :].rearrange("p (m t) -> p m t", m=2)
        Vt = carve("Vt", NT * 258)
        V_ap = Vt[:, 0:NT * 258].rearrange("p (t e) -> p t e", e=258)
        bias_c = carve("bias_c", 1024, F32)
        cc_c = carve("cc_c", 128, F32)
        tmpf = [carve("tmpf%d" % i, 512, F32) for i in range(3)]
        ptd = [carve("ptd%d" % i, 512) for i in range(4)]
        accA = [carve("accA%d" % i, 256, F32) for i in range(4)]
        od = [carve("od%d" % i, 256, F32) for i in range(2)]
        odb = [carve("odb%d" % i, 256) for i in range(2)]
        rsd = [carve("rsd%d" % i, 4, F32) for i in range(2)]
        dstg = [carve("dstg%d" % i, 2 * 512) for i in range(2)]
        P.dma("sp", cc_c[:, 0:128], consts[:, C_CC:C_CC + 128], W=[cc_c])
        P.add("dve", f_memset(Vt[:, :], 1.0), W=[Vt])
        sc_d = 128 ** -0.5
        OB = (0, 1, 2, 3)
        SB_ = (4, 5, 6)
        TB_ = (7,)
        PB_ = (4, 5, 6, 7)
        for h in range(4):
            load_w(wd_c, wd_ap[:, :, 0:256], w_in[l], OFF_DQ + h * 256, 256)
            load_w(wd_c, wd_ap[:, :, 256:512], w_in[l], OFF_DK + h * 256, 256)
            load_w(wd_c, wd_ap[:, :, 512:768], w_in[l], OFF_DV + h * 256, 256)
            P.dma("sp", bias_c[:, 0:1024], consts[:, C_B + h * 1024:C_B + (h + 1) * 1024], W=[bias_c])
            for tb in range(NB):
                for c in range(4):
                    b = bank(PB_)
                    for kc in range(8):
                        a, tl = hT_blk(kc, tb)
                        P.add("pe", f_mm(b[:, 0:512], wd_ap[:, kc, c * 128:(c + 1) * 128], a, kc == 0, kc == 7), R=[wd_c] + tl, W=[b])
                    dst_t = dqT if c < 2 else dkT
                    dst_ap = (dqT_ap if c < 2 else dkT_ap)[:, c % 2, tb * 512:(tb + 1) * 512]
                    copy_on(evac_eng(), dst_ap, b[:, 0:512], [b], [dst_t])
            for t in range(NT):
                b = bank(PB_)
                for kc in range(8):
                    a, tl = hT_tok(kc, t)
                    P.add("pe", f_mm(b[:, 0:256], a, wd_ap[:, kc, 512:768], kc == 0, kc == 7), R=[wd_c] + tl, W=[b])
                copy_on(evac_eng(), V_ap[:, t, 0:256], b[:, 0:256], [b], [Vt])
            tiles = [(g, m, j) for g in range(NB) for m in range(2) for j in range(4 * g + 4)]
            LOOK = 2
            pend = {}

            def emit_qk(idx):
                g, m, j = tiles[idx]
                r = j - 4 * g
                q0c = max(r, 0) * 128
                nq = 512 - q0c
                b = bank(SB_)
                P.add("pe", f_mm(b[:, 0:nq], dkT_ap[:, m, j * 128:(j + 1) * 128], dqT_ap[:, m, g * 512 + q0c:(g + 1) * 512], True, True), R=[dkT, dqT], W=[b])
                tf = tmpf[idx % 3]
                pt = ptd[idx % 4]
                if r >= 0:
                    P.add("dve", f_stt(tf[:, 0:nq], b[:, 0:nq], sc_d, bias_c[:, 512:512 + nq], ALU.mult, ALU.add), R=[b, bias_c], W=[tf])
                    P.add("act", f_act(pt[:, 0:nq], tf[:, 0:nq], AF.Exp), R=[tf], W=[pt])
                else:
                    P.add("dve", f_stt(tf[:, 0:nq], b[:, 0:nq], sc_d, bias_c[:, 0:nq], ALU.mult, ALU.add), R=[b, bias_c], W=[tf])
                    dt_ = 4 * g - j
                    P.add("act", f_act(pt[:, 0:nq], tf[:, 0:nq], AF.Exp, bias=cc_c[:, h * 32 + dt_:h * 32 + dt_ + 1]), R=[tf, cc_c], W=[pt])
                pend[idx] = (pt, r, q0c)

            def emit_pv(idx):
                g, m, j = tiles[idx]
                pt, r, q0c = pend.pop(idx)
                for qt in range(max(r, 0), 4):
                    last_j = 4 * g + qt
                    P.add("pe", f_mm(obs[qt][:, 0:258], pt[:, qt * 128 - q0c:(qt + 1) * 128 - q0c], V_ap[:, j, :], j == 0, j == last_j), R=[pt, Vt], W=[obs[qt]])

            def finish_pass(g, m):
                dst_ = dstg[g % 2]
                dst_ap2 = dst_[:, :].rearrange("p (c t) -> p c t", c=2)
                for qt in range(4):
                    ob = obs[qt]
                    rs = rsd[qt % 2]
                    P.add("dve", lambda e, o=rs[:, 0:1], i=ob[:, 256:257]: e.reciprocal(out=o, in_=i), R=[ob], W=[rs])
                    if m == 0:
                        P.add("dve", f_ts(accA[qt][:, 0:256], ob[:, 0:256], rs[:, 0:1], None, ALU.mult), R=[ob, rs], W=[accA[qt]])
                    else:
                        P.add("dve", f_tt(rs[:, 1:2], rs[:, 0:1], lamt[:, 5:6], ALU.mult), R=[rs, lamt], W=[rs])
                        o_ = od[qt % 2]
                        P.add("dve", f_stt(o_[:, 0:256], ob[:, 0:256], rs[:, 1:2], accA[qt][:, 0:256], ALU.mult, ALU.add), R=[ob, rs, accA[qt]], W=[o_])
                        ss = sst[qt % 3]
                        P.add("act", f_act(junk[:, 0:256], o_[:, 0:256], AF.Square, accum_out=ss[:, 0:1]), R=[o_], W=[junk, ss])
                        rstd_from(ss, ss[:, 0:1], 256, [ss])
                        ob_ = odb[qt % 2]
                        P.add("dve", f_ts(ob_[:, 0:256], o_[:, 0:256], ss[:, 2:3], None, ALU.mult), R=[o_, ss], W=[ob_])
                        b2 = bank(TB_)
                        pb = psb(b2)
                        for c in range(2):
                            P.add("pe", f_tr(pb[:, c * 128:(c + 1) * 128], ob_[:, c * 128:(c + 1) * 128], ident[:]), R=[ob_, ident], W=[b2])
                        copy_on("act", dst_ap2[:, :, qt * 128:(qt + 1) * 128], pb[:, 0:256].rearrange("p (c t) -> p c t", c=2), [b2], [dst_])
                if m == 1:
                    dd = daT[h * 256:(h + 1) * 256, g * 512:(g + 1) * 512].rearrange("(c p) t -> p c t", p=128)
                    P.dma("sp", dd, dst_ap2, R=[dst_], W=[d_daT])

            obs = [ps[i] for i in OB]
            nt_ = len(tiles)
            for idx in range(min(LOOK, nt_)):
                emit_qk(idx)
            for idx in range(nt_):
                if idx + LOOK < nt_:
                    emit_qk(idx + LOOK)
                emit_pv(idx)
                g, m, j = tiles[idx]
                if j == 4 * g + 3:
                    finish_pass(g, m)
        _phase_end(5)

        o2[0] = 0
        wro = carve("wro", 16 * 1024)
        wdo = carve("wdo", 8 * 1024)
        wco = carve("wco", 8 * 1024)
        wou = carve("wou", 8 * 1024)
        wro_ap = wro[:, :].rearrange("p (c n) -> p c n", c=16)
        wdo_ap = wdo[:, :].rearrange("p (c n) -> p c n", c=8)
        wco_ap = wco[:, :].rearrange("p (c n) -> p c n", c=8)
        wou_ap = wou[:, :].rearrange("p (c n) -> p c n", c=8)
        load_w(wro, wro_ap[:, 0:8, :], w_ret_o[l][0:1024, :], 0, 1024)
        load_w(wro, wro_ap[:, 8:16, :], w_ret_o[l][1024:2048, :], 0, 1024)
        load_w(wdo, wdo_ap, w_diff_o[l], 0, 1024)
        load_w(wco, wco_ap, w_cross_o[l], 0, 1024)
        load_w(wou, wou_ap, w_out[l], 0, 1024)
        gcol = carve("gcol", 32, F32)
        P.dma("sp", gcol[:, 0:16], g_ret[l].rearrange("(c p) -> p c", p=128), W=[gcol], slow=True)
        P.dma("sp", gcol[:, 16:24], g_diff[l].rearrange("(c p) -> p c", p=128), W=[gcol], slow=True)
        P.add("dve", f_ts(gcol[:, 16:24], gcol[:, 16:24], 1.0 - lam_init, None, ALU.mult), R=[gcol], W=[gcol])
        for c in range(16):
            P.add("dve", f_ts(wro_ap[:, c, :], wro_ap[:, c, :], gcol[:, c:c + 1], None, ALU.mult), R=[wro, gcol], W=[wro])
        for c in range(8):
            P.add("dve", f_ts(wdo_ap[:, c, :], wdo_ap[:, c, :], gcol[:, 16 + c:17 + c], None, ALU.mult), R=[wdo, gcol], W=[wdo])
        inb = [carve("inb%d" % i, 16 * 512) for i in range(2)]
        inb2 = [carve("inc%d" % i, 16 * 512) for i in range(2)]
        gb_ = [carve("gb%d" % i, 3 * 512) for i in range(2)]
        mT = carve("mT", 8 * 512)
        mT_ap = mT[:, :].rearrange("p (c t) -> p c t", c=8)
        mf = [carve("mf%d" % i, 512, F32) for i in range(2)]
        mf2 = [carve("mg%d" % i, 512, F32) for i in range(2)]
        gi = 0
        for tb in range(NB):
            a_ = inb[tb % 2]
            c_ = inb2[tb % 2]
            a_ap = a_[:, :].rearrange("p (c t) -> p c t", c=16)
            c_ap = c_[:, :].rearrange("p (c t) -> p c t", c=16)
            P.dma("sp", a_ap, retT[:, tb * 512:(tb + 1) * 512].rearrange("(c p) t -> p c t", p=128), R=[d_retT], W=[a_])
            P.dma("sp", c_ap[:, 0:8, :], daT[:, tb * 512:(tb + 1) * 512].rearrange("(c p) t -> p c t", p=128), R=[d_daT], W=[c_])
            P.dma("sp", c_ap[:, 8:16, :], caT[:, tb * 512:(tb + 1) * 512].rearrange("(c p) t -> p c t", p=128), R=[d_caT], W=[c_])
            for cc in range(8):
                g_ = gb_[gi % 2]
                g_ap = g_[:, :].rearrange("p (b t) -> p b t", b=3)
                for br in range(3):
                    P.dma("sp", g_ap[:, br, :], gT[br * 1024 + cc * 128:br * 1024 + (cc + 1) * 128, tb * 512:(tb + 1) * 512], R=[d_gT], W=[g_])
                br_ = bank()
                for fc in range(16):
                    P.add("pe", f_mm(br_[:, 0:512], wro_ap[:, fc, cc * 128:(cc + 1) * 128], a_ap[:, fc, :], fc == 0, fc == 15), R=[wro, a_], W=[br_])
                bd_ = bank()
                for fc in range(8):
                    P.add("pe", f_mm(bd_[:, 0:512], wdo_ap[:, fc, cc * 128:(cc + 1) * 128], c_ap[:, fc, :], fc == 0, fc == 7), R=[wdo, c_], W=[bd_])
                bc_ = bank()
                for fc in range(8):
                    P.add("pe", f_mm(bc_[:, 0:512], wco_ap[:, fc, cc * 128:(cc + 1) * 128], c_ap[:, 8 + fc, :], fc == 0, fc == 7), R=[wco, c_], W=[bc_])
                m1, m2 = mf[gi % 2], mf2[gi % 2]
                gi += 1
                P.add("dve", f_tt(m1[:, 0:512], br_[:, 0:512], g_ap[:, 0, :], ALU.mult), R=[br_, g_], W=[m1])
                P.add("dve", f_tt(m2[:, 0:512], bd_[:, 0:512], g_ap[:, 1, :], ALU.mult), R=[bd_, g_], W=[m2])
                P.add("dve", f_tt(m1[:, 0:512], m1[:, 0:512], m2[:, 0:512], ALU.add), R=[m1, m2], W=[m1])
                P.add("dve", f_tt(m2[:, 0:512], bc_[:, 0:512], g_ap[:, 2, :], ALU.mult), R=[bc_, g_], W=[m2])
                P.add("dve", f_tt(mT_ap[:, cc, :], m1[:, 0:512], m2[:, 0:512], ALU.add), R=[m1, m2], W=[mT])
            for qt in range(4):
                t = tb * 4 + qt
                xt = xts[t % 3]
                P.dma("sp", xt[:], xsrc[t * 128:(t + 1) * 128, :], R=[d_xres], W=[xt])
                for half in range(2):
                    b = bank()
                    for cc in range(8):
                        P.add("pe", f_mm(b[:, 0:512], mT_ap[:, cc, qt * 128:(qt + 1) * 128], wou_ap[:, cc, half * 512:(half + 1) * 512], cc == 0, cc == 7), R=[mT, wou], W=[b])
                    P.add("dve", f_tt(xt[:, half * 512:(half + 1) * 512], b[:, 0:512], xt[:, half * 512:(half + 1) * 512], ALU.add), R=[b, xt], W=[xt])
                P.dma("sp", xres[t * 128:(t + 1) * 128, :], xt[:, 0:1024], R=[xt], W=[d_xres])
        _phase_end(6)

        o2[0] = 0
        wup = carve("wup", 32768)
        wdn = carve("wdn", 32768)
        wup_ap = wup[:, :].rearrange("p (c n) -> p c n", c=8)
        wdn_ap = wdn[:, :].rearrange("p (c n) -> p c n", c=32)
        for q in range(4):
            load_w(wup, wup_ap[:, :, q * 1024:(q + 1) * 1024], w_up[l], q * 1024, 1024)
        for q in range(4):
            load_w(wdn, wdn_ap[:, q * 8:(q + 1) * 8, :], w_down[l][q * 1024:(q + 1) * 1024, :], 0, 1024)
        h2T = [carve("h2T%d" % i, 8 * 512) for i in range(1)]
        uT = carve("uT", 32 * 512)
        uT_ap = uT[:, :].rearrange("p (c t) -> p c t", c=32)
        rl = [carve("rl%d" % i, 512) for i in range(3)]
        gfin = carve("gfin", 1024, F32)
        last = (l == NL - 1)
        P.dma("sp", gbc[:], g_ffn[l:l + 1, :].broadcast_to([128, D]), W=[gbc])
        if last:
            P.dma("sp", gfin[:, 0:1024], g_final[0:1, :].broadcast_to([128, D]), W=[gfin])
        ri = 0
        for tb in range(NB):
            h2 = h2T[0]
            h2_ap = h2[:, :].rearrange("p (c t) -> p c t", c=8)
            for qt in range(4):
                t = tb * 4 + qt
                xt, ss, hb = xts[t % 3], sst[t % 3], hbs[t % 2]
                P.dma("sp", xt[:], xres[t * 128:(t + 1) * 128, :], R=[d_xres], W=[xt])
                norm_tile(xt, ss, gbc, hb)
                transpose_to(hb, 8, h2_ap[:, :, qt * 128:(qt + 1) * 128], h2, eng="act")
            for fc in range(32):
                b = bank()
                for kc in range(8):
                    P.add("pe", f_mm(b[:, 0:512], wup_ap[:, kc, fc * 128:(fc + 1) * 128], h2_ap[:, kc, :], kc == 0, kc == 7), R=[wup, h2], W=[b])
                r_ = rl[ri % 3]
                ri += 1
                P.add("dve", f_ts(r_[:, 0:512], b[:, 0:512], 0.0, None, ALU.max), R=[b], W=[r_])
                if fc % 2:
                    P.add("pool", f_tt(uT_ap[:, fc, :], r_[:, 0:512], r_[:, 0:512], ALU.mult), R=[r_], W=[uT])
                else:
                    P.add("act", f_act(uT_ap[:, fc, :], r_[:, 0:512], AF.Square), R=[r_], W=[uT])
            for qt in range(4):
                t = tb * 4 + qt
                xt = xts[t % 3]
                P.dma("sp", xt[:], xres[t * 128:(t + 1) * 128, :], R=[d_xres], W=[xt])
                for half in range(2):
                    b = bank()
                    for fc in range(32):
                        P.add("pe", f_mm(b[:, 0:512], uT_ap[:, fc, qt * 128:(qt + 1) * 128], wdn_ap[:, fc, half * 512:(half + 1) * 512], fc == 0, fc == 31), R=[uT, wdn], W=[b])
                    P.add("dve", f_tt(xt[:, half * 512:(half + 1) * 512], b[:, 0:512], xt[:, half * 512:(half + 1) * 512], ALU.add), R=[b, xt], W=[xt])
                if not last:
                    P.dma("sp", xres[t * 128:(t + 1) * 128, :], xt[:, 0:1024], R=[xt], W=[d_xres])
                else:
                    ss = sst[t % 3]
                    P.add("act", f_act(junk[:, 0:1024], xt[:, 0:1024], AF.Square, accum_out=ss[:, 0:1]), R=[xt], W=[junk, ss])
                    rstd_from(ss, ss[:, 0:1], 1024, [ss])
                    P.add("dve", f_stt(xt[:, 0:1024], xt[:, 0:1024], ss[:, 2:3], gfin[:, 0:1024], ALU.mult, ALU.mult), R=[xt, ss, gfin], W=[xt])
                    P.dma("sp", y_out[t * 128:(t + 1) * 128, :], xt[:, 0:1024], R=[xt], W=[d_y])
        _phase_end(7)

      except _Stop:
        break
    P.emit()
    es.close()
    return nc


_CACHE = {}


def kernel(**inputs):
    x = np.asarray(inputs["x"])
    B, S, _ = x.shape
    key = (S,)
    if key not in _CACHE:
        _CACHE[key] = build(S, 2)
    nc = _CACHE[key]
    consts = make_consts()
    shared = {k: np.ascontiguousarray(np.asarray(v)) for k, v in inputs.items() if k not in ("x", "mem")}
    shared["g_final"] = shared["g_final"].reshape(1, D)
    shared["consts"] = consts
    mem = np.asarray(inputs["mem"])
    in_maps = []
    for b in range(B):
        m = dict(shared)
        m["x"] = np.ascontiguousarray(x[b])
        m["mem"] = np.ascontiguousarray(mem[b])
        in_maps.append(m)
    res = run_bass_kernel_spmd(nc, in_maps, core_ids=list(range(B)))
    return np.stack([r["y"] for r in res.results], axis=0).astype(np.float32)
```
